# Optimizing a Trainium2 kernel written in Bass

```python
import jax
import jax.numpy as jnp
from jax import lax
import numpy as np

D_MODEL = 4096
BATCH = 8
SEQ = 2048
DEPTH = 2

GRID_W = 64
CTX_LEN = 256
Q_BLOCK = 128
ROPE_THETA = 10000.0
NORM_EPS = 1e-6

HEAD_DIM = 128
GQA_HEADS = 16
GQA_KV_HEADS = 4
GQA_GROUP = GQA_HEADS // GQA_KV_HEADS
GQA_Q_W = GQA_HEADS * HEAD_DIM
GQA_KV_W = GQA_KV_HEADS * HEAD_DIM
GQA_SCALE = HEAD_DIM ** -0.5

MLA_HEADS = 8
MLA_Q_LORA = 1024
MLA_KV_LORA = 512
MLA_NOPE_DIM = 128
MLA_ROPE_DIM = 64
MLA_V_DIM = 128
MLA_QK_DIM = MLA_NOPE_DIM + MLA_ROPE_DIM
MLA_OUT_W = MLA_HEADS * MLA_V_DIM
MLA_SCALE = MLA_QK_DIM ** -0.5

FOURIER_GROUPS = 4
FOURIER_GROUP_W = 256
FOURIER_W = FOURIER_GROUPS * FOURIER_GROUP_W

N_BRANCHES = 3

COL_SIZES = (GQA_KV_W, GQA_KV_W, MLA_KV_LORA, MLA_ROPE_DIM, GQA_Q_W, MLA_Q_LORA, FOURIER_W, N_BRANCHES * D_MODEL)
KV_COLS = 2 * GQA_KV_W + MLA_KV_LORA + MLA_ROPE_DIM
IN_COLS = KV_COLS + GQA_Q_W + MLA_Q_LORA + FOURIER_W + N_BRANCHES * D_MODEL

N_EXPERTS = 32
TOP_K = 4
EXPERT_FF = 512
SWIGLU_ALPHA = 1.702
SWIGLU_LIMIT = 7.0

kernel_name = 'hybrid_gqa_mla_fourier_moe_dit_block'


def rmsnorm(x, gain):
    xf = x.astype(jnp.float32)
    y = xf * lax.rsqrt(jnp.mean(xf * xf, axis=-1, keepdims=True) + NORM_EPS)
    return (y * gain.astype(jnp.float32)).astype(x.dtype)


def split_widths(z, widths):
    cuts = []
    total = 0
    for w in widths[:-1]:
        total += w
        cuts.append(total)
    return jnp.split(z, cuts, axis=-1)


def axial_rope_tables(row, col, rot_dim):
    half = rot_dim // 2
    inv = 1.0 / (ROPE_THETA ** (jnp.arange(0, half, 2, dtype=jnp.float32) / half))
    ang_r = row[:, None] * inv[None, :]
    ang_c = col[:, None] * inv[None, :]
    return (jnp.cos(ang_r), jnp.sin(ang_r), jnp.cos(ang_c), jnp.sin(ang_c))


def _rotate(x, cos, sin):
    d2 = x.shape[-1] // 2
    x1, x2 = x[..., :d2], x[..., d2:]
    c = cos[:, None, :]
    s = sin[:, None, :]
    return jnp.concatenate([x1 * c - x2 * s, x1 * s + x2 * c], axis=-1)


def apply_axial_rope(x, tables):
    cos_r, sin_r, cos_c, sin_c = tables
    xf = x.astype(jnp.float32)
    half = x.shape[-1] // 2
    out = jnp.concatenate([_rotate(xf[..., :half], cos_r, sin_r), _rotate(xf[..., half:], cos_c, sin_c)], axis=-1)
    return out.astype(x.dtype)


def blocked_attention(q, k, v, scale):
    b, nq, hk, g, dq = q.shape
    qb = jnp.moveaxis(q.reshape(b, nq // Q_BLOCK, Q_BLOCK, hk, g, dq), 1, 0)

    def one_block(qi):
        s = jnp.einsum('bqhgd,bshd->bhgqs', qi, k).astype(jnp.float32) * scale
        p = jax.nn.softmax(s, axis=-1).astype(v.dtype)
        return jnp.einsum('bhgqs,bshd->bqhgd', p, v)

    o = jnp.moveaxis(lax.map(one_block, qb), 0, 1)
    return o.reshape(b, nq, -1)


def gqa_q(zq, q_norm, rope):
    b, n, _ = zq.shape
    q = rmsnorm(zq.reshape(b, n, GQA_HEADS, HEAD_DIM), q_norm)
    if rope is not None:
        q = apply_axial_rope(q, rope)
    return q.reshape(b, n, GQA_KV_HEADS, GQA_GROUP, HEAD_DIM)


def gqa_kv(zk, zv, k_norm, rope):
    b, n, _ = zk.shape
    k = rmsnorm(zk.reshape(b, n, GQA_KV_HEADS, HEAD_DIM), k_norm)
    if rope is not None:
        k = apply_axial_rope(k, rope)
    return k, zv.reshape(b, n, GQA_KV_HEADS, HEAD_DIM)


def mla_q(zcq, q_norm, w_uq, rope):
    b, n, _ = zcq.shape
    q = (rmsnorm(zcq, q_norm) @ w_uq).reshape(b, n, MLA_HEADS, MLA_QK_DIM)
    q_nope, q_rope = q[..., :MLA_NOPE_DIM], q[..., MLA_NOPE_DIM:]
    if rope is not None:
        q_rope = apply_axial_rope(q_rope, rope)
    return jnp.concatenate([q_nope, q_rope], axis=-1)[:, :, :, None, :]


def mla_kv(zckv, zkr, kv_norm, w_ukv, rope):
    b, n, _ = zckv.shape
    kv = (rmsnorm(zckv, kv_norm) @ w_ukv).reshape(b, n, MLA_HEADS, MLA_NOPE_DIM + MLA_V_DIM)
    k_nope, v = kv[..., :MLA_NOPE_DIM], kv[..., MLA_NOPE_DIM:]
    k_rope = zkr[:, :, None, :]
    if rope is not None:
        k_rope = apply_axial_rope(k_rope, rope)
    k_rope = jnp.broadcast_to(k_rope, (b, n, MLA_HEADS, MLA_ROPE_DIM))
    return jnp.concatenate([k_nope, k_rope], axis=-1), v


def fourier_mix(zf):
    b, n, _ = zf.shape
    zg = zf.astype(jnp.float32).reshape(b, n, FOURIER_GROUPS, FOURIER_GROUP_W)
    y = jnp.fft.fft2(zg, axes=(1, 3), norm='ortho').real
    return y.reshape(b, n, FOURIER_W).astype(zf.dtype)


def merge_branches(y_gqa, y_mla, y_fft, z_gate, w_br_gqa, w_br_mla, w_br_fourier, w_out):
    g_gqa, g_mla, g_fft = jnp.split(jax.nn.sigmoid(z_gate), N_BRANCHES, axis=-1)
    m = g_gqa * (y_gqa @ w_br_gqa) + g_mla * (y_mla @ w_br_mla) + g_fft * (y_fft @ w_br_fourier)
    return m @ w_out


def clamped_swiglu(hgu):
    g = jnp.minimum(hgu[..., 0::2], SWIGLU_LIMIT)
    lin = jnp.clip(hgu[..., 1::2], -SWIGLU_LIMIT, SWIGLU_LIMIT)
    return g * jax.nn.sigmoid(SWIGLU_ALPHA * g) * (lin + 1.0)


def moe(t, w_router, b_router, w_gate_up, b_gate_up, w_down, b_down):
    logits = (t @ w_router + b_router).astype(jnp.float32)
    top_val, top_idx = lax.top_k(logits, TOP_K)
    top_w = jax.nn.softmax(top_val, axis=-1)
    comb = jnp.sum(jax.nn.one_hot(top_idx, N_EXPERTS, dtype=jnp.float32) * top_w[..., None], axis=1).astype(t.dtype)
    out = jnp.zeros_like(t)
    for e in range(N_EXPERTS):
        y = clamped_swiglu(t @ w_gate_up[e] + b_gate_up[e]) @ w_down[e] + b_down[e]
        out = out + comb[:, e:e + 1] * y
    return out


def trunk_layer(h, hc, mod, mod_c, rope_gqa, rope_mla, norm_mix, w_in, gqa_q_norm, gqa_k_norm,
                mla_q_norm, mla_w_uq, mla_kv_norm, mla_w_ukv, w_br_gqa, w_br_mla, w_br_fourier,
                w_out, norm_ffn, w_router, b_router, w_gate_up, b_gate_up, w_down, b_down, last):
    sh1, sc1, g1, sh2, sc2, g2 = jnp.split(mod[:, None, :], 6, axis=-1)
    sh1c, sc1c, g1c, sh2c, sc2c, g2c = jnp.split(mod_c[:, None, :], 6, axis=-1)

    u = rmsnorm(h, norm_mix) * (1.0 + sc1) + sh1
    uc = rmsnorm(hc, norm_mix) * (1.0 + sc1c) + sh1c
    z = u @ w_in
    zc = uc @ (w_in[:, :KV_COLS] if last else w_in)
    zk_a, zv_a, zckv, zkr, zq_a, zcq, zf, zg = split_widths(z, COL_SIZES)
    zk_ac, zv_ac, zckv_c, zkr_c = split_widths(zc[..., :KV_COLS], COL_SIZES[:4])

    k_a_lat, v_a_lat = gqa_kv(zk_a, zv_a, gqa_k_norm, rope_gqa)
    k_a_ctx, v_a_ctx = gqa_kv(zk_ac, zv_ac, gqa_k_norm, None)
    y_a = blocked_attention(gqa_q(zq_a, gqa_q_norm, rope_gqa),
                            jnp.concatenate([k_a_ctx, k_a_lat], axis=1),
                            jnp.concatenate([v_a_ctx, v_a_lat], axis=1), GQA_SCALE)

    k_b_lat, v_b_lat = mla_kv(zckv, zkr, mla_kv_norm, mla_w_ukv, rope_mla)
    k_b_ctx, v_b_ctx = mla_kv(zckv_c, zkr_c, mla_kv_norm, mla_w_ukv, None)
    y_b = blocked_attention(mla_q(zcq, mla_q_norm, mla_w_uq, rope_mla),
                            jnp.concatenate([k_b_ctx, k_b_lat], axis=1),
                            jnp.concatenate([v_b_ctx, v_b_lat], axis=1), MLA_SCALE)

    y_c = fourier_mix(zf)

    h = h + g1 * merge_branches(y_a, y_b, y_c, zg, w_br_gqa, w_br_mla, w_br_fourier, w_out)

    if not last:
        _, _, _, _, zq_ac, zcq_c, zf_c, zg_c = split_widths(zc, COL_SIZES)
        yc_a = blocked_attention(gqa_q(zq_ac, gqa_q_norm, None), k_a_ctx, v_a_ctx, GQA_SCALE)
        yc_b = blocked_attention(mla_q(zcq_c, mla_q_norm, mla_w_uq, None), k_b_ctx, v_b_ctx, MLA_SCALE)
        yc_c = fourier_mix(zf_c)
        hc = hc + g1c * merge_branches(yc_a, yc_b, yc_c, zg_c, w_br_gqa, w_br_mla, w_br_fourier, w_out)

    b, n, d = h.shape
    v = rmsnorm(h, norm_ffn) * (1.0 + sc2) + sh2
    if last:
        f = moe(v.reshape(b * n, d), w_router, b_router, w_gate_up, b_gate_up, w_down, b_down)
        h = h + g2 * f.reshape(b, n, d)
    else:
        vc = rmsnorm(hc, norm_ffn) * (1.0 + sc2c) + sh2c
        tokens = jnp.concatenate([v.reshape(b * n, d), vc.reshape(-1, d)], axis=0)
        f = moe(tokens, w_router, b_router, w_gate_up, b_gate_up, w_down, b_down)
        h = h + g2 * f[:b * n].reshape(b, n, d)
        hc = hc + g2c * f[b * n:].reshape(hc.shape)
    return h, hc


def setup_inputs(seed: int = 0) -> dict:
    key = jax.random.key(seed)
    k = jax.random.split(key, 26)
    f32 = jnp.float32

    def normal(kk, shape, std):
        return jax.random.normal(kk, shape, f32) * std

    def gain(kk, shape):
        return 1.0 + 0.02 * jax.random.normal(kk, shape, f32)

    D = D_MODEL
    return {
        'x': normal(k[0], (BATCH, SEQ, D), 1.0),
        'c': normal(k[1], (BATCH, D), 1.0),
        'ctx': normal(k[2], (BATCH, CTX_LEN, D), 1.0),
        'c_ctx': normal(k[3], (D,), 1.0),
        'w_ada': normal(k[4], (DEPTH, D, 6 * D), 0.5 * D ** -0.5),
        'b_ada': normal(k[5], (DEPTH, 6 * D), 0.02),
        'norm_mix': gain(k[6], (DEPTH, D)),
        'w_in': normal(k[7], (DEPTH, D, IN_COLS), D ** -0.5),
        'gqa_q_norm': gain(k[8], (DEPTH, HEAD_DIM)),
        'gqa_k_norm': gain(k[9], (DEPTH, HEAD_DIM)),
        'mla_q_norm': gain(k[10], (DEPTH, MLA_Q_LORA)),
        'mla_w_uq': normal(k[11], (DEPTH, MLA_Q_LORA, MLA_HEADS * MLA_QK_DIM), MLA_Q_LORA ** -0.5),
        'mla_kv_norm': gain(k[12], (DEPTH, MLA_KV_LORA)),
        'mla_w_ukv': normal(k[13], (DEPTH, MLA_KV_LORA, MLA_HEADS * (MLA_NOPE_DIM + MLA_V_DIM)), MLA_KV_LORA ** -0.5),
        'w_br_gqa': normal(k[14], (DEPTH, GQA_Q_W, D), GQA_Q_W ** -0.5),
        'w_br_mla': normal(k[15], (DEPTH, MLA_OUT_W, D), MLA_OUT_W ** -0.5),
        'w_br_fourier': normal(k[16], (DEPTH, FOURIER_W, D), FOURIER_W ** -0.5),
        'w_out': normal(k[17], (DEPTH, D, D), D ** -0.5),
        'norm_ffn': gain(k[18], (DEPTH, D)),
        'w_router': normal(k[19], (DEPTH, D, N_EXPERTS), D ** -0.5),
        'b_router': normal(k[20], (DEPTH, N_EXPERTS), 0.01),
        'w_gate_up': normal(k[21], (DEPTH, N_EXPERTS, D, 2 * EXPERT_FF), D ** -0.5),
        'b_gate_up': normal(k[22], (DEPTH, N_EXPERTS, 2 * EXPERT_FF), 0.01),
        'w_down': normal(k[23], (DEPTH, N_EXPERTS, EXPERT_FF, D), EXPERT_FF ** -0.5),
        'b_down': normal(k[24], (DEPTH, N_EXPERTS, D), 0.01),
        'norm_final': gain(k[25], (D,)),
    }


def reference(x, c, ctx, c_ctx, w_ada, b_ada, norm_mix, w_in, gqa_q_norm, gqa_k_norm, mla_q_norm,
              mla_w_uq, mla_kv_norm, mla_w_ukv, w_br_gqa, w_br_mla, w_br_fourier, w_out, norm_ffn,
              w_router, b_router, w_gate_up, b_gate_up, w_down, b_down, norm_final):
    ROWS = x.shape[1] // GRID_W
    row = jnp.repeat(jnp.arange(ROWS, dtype=jnp.float32), GRID_W)
    col = jnp.tile(jnp.arange(GRID_W, dtype=jnp.float32), ROWS)
    rope_gqa = axial_rope_tables(row, col, HEAD_DIM)
    rope_mla = axial_rope_tables(row, col, MLA_ROPE_DIM)
    c_act = jax.nn.silu(c)
    c_ctx_act = jax.nn.silu(c_ctx)[None, :]
    h, hc = x, ctx
    for l in range(DEPTH):
        mod = c_act @ w_ada[l] + b_ada[l]
        mod_c = c_ctx_act @ w_ada[l] + b_ada[l]
        h, hc = trunk_layer(h, hc, mod, mod_c, rope_gqa, rope_mla, norm_mix[l], w_in[l],
                            gqa_q_norm[l], gqa_k_norm[l], mla_q_norm[l], mla_w_uq[l], mla_kv_norm[l],
                            mla_w_ukv[l], w_br_gqa[l], w_br_mla[l], w_br_fourier[l], w_out[l],
                            norm_ffn[l], w_router[l], b_router[l], w_gate_up[l], b_gate_up[l],
                            w_down[l], b_down[l], l == DEPTH - 1)
    return rmsnorm(h, norm_final)
```

```python
import numpy as np
import ml_dtypes
import concourse.bass as bass
import concourse.mybir as mybir
from concourse.bass_utils import run_bass_kernel_spmd

F32 = mybir.dt.float32
BF16 = mybir.dt.bfloat16
ALU = mybir.AluOpType
AF = mybir.ActivationFunctionType

D = 4096
KC = 32
NCTX = 256
SEQ = 2048
T = NCTX + SEQ
TT = [(0, 256), (256, 512), (768, 512), (1280, 512), (1792, 512)]
NL = 2
EPS = 1e-6
IN_COLS = 17984
NE = 32
GQA_SCALE = 128 ** -0.5
MLA_SCALE = 192 ** -0.5


class _Op:
    __slots__ = ("eng", "fn", "deps", "dma", "sem", "target", "need_inc", "prev_slot")

    def __init__(self, eng, fn, dma):
        self.eng = eng
        self.fn = fn
        self.dma = dma
        self.deps = []
        self.sem = None
        self.target = 0
        self.need_inc = False
        self.prev_slot = None


class Sched:
    ENGS = ("pe", "act", "dve", "pool", "sp")
    NDMA = {"sp": 20, "pool": 12, "act": 4}

    def __init__(self, nc):
        self.nc = nc
        self.ops = {e: [] for e in self.ENGS}
        self.lastw = {}
        self.readers = {}

    def add(self, eng, fn, reads=(), writes=(), dma=False):
        op = _Op(eng, fn, dma)
        deps = {}
        for k in reads:
            w = self.lastw.get(k)
            if w is not None:
                deps[id(w)] = w
        for k in writes:
            w = self.lastw.get(k)
            if w is not None:
                deps[id(w)] = w
            for r in self.readers.get(k, {}).values():
                deps[id(r)] = r
        rk = id(op) if dma else eng
        for k in reads:
            self.readers.setdefault(k, {})[rk] = op
        for k in writes:
            self.lastw[k] = op
            self.readers[k] = {}
        for d in deps.values():
            if (not d.dma) and (not dma) and d.eng == "pe" and eng == "pe":
                continue
            op.deps.append(d)
        self.ops[eng].append(op)
        return op

    def dma(self, q, out, in_, reads=(), writes=()):
        return self.add(q, lambda e: e.dma_start(out=out, in_=in_), reads, writes, dma=True)

    def emit(self):
        nc = self.nc
        esem = {e: nc.alloc_semaphore("s_" + e) for e in ("pe", "act", "dve", "pool")}
        dsem = {q: [nc.alloc_semaphore("d_%s%d" % (q, i)) for i in range(n)] for q, n in self.NDMA.items()}
        for e in self.ENGS:
            for op in self.ops[e]:
                for d in op.deps:
                    d.need_inc = True
        for e in self.ENGS:
            cnt = 0
            nd = 0
            slot_last = {}
            for op in self.ops[e]:
                if op.dma:
                    sl = nd % len(dsem[e])
                    nd += 1
                    op.sem = dsem[e][sl]
                    op.prev_slot = slot_last.get(sl)
                    op.target = (op.prev_slot.target if op.prev_slot is not None else 0) + 16
                    slot_last[sl] = op
                else:
                    op.sem = esem[e]
                    if op.need_inc:
                        cnt += 1
                    op.target = cnt
        ops = self.ops

        def run(ename, eng):
            waited = {}
            last = {}
            for op in ops[ename]:
                need = {}
                for d in op.deps:
                    k = id(d.sem)
                    if k not in need or need[k][1] < d.target:
                        need[k] = (d.sem, d.target)
                if op.dma and op.prev_slot is not None:
                    k = id(op.sem)
                    t = op.prev_slot.target
                    if k not in need or need[k][1] < t:
                        need[k] = (op.sem, t)
                for k, (s, t) in need.items():
                    if waited.get(k, 0) >= t:
                        continue
                    eng.wait_ge(s, t)
                    waited[k] = t
                ins = op.fn(eng)
                if op.dma:
                    ins.then_inc(op.sem, 16)
                    last[id(op.sem)] = (op.sem, op.target)
                elif op.need_inc:
                    ins.then_inc(op.sem, 1)
            for k, (s, t) in last.items():
                if waited.get(k, 0) < t:
                    eng.wait_ge(s, t)

        with nc.Block() as block:
            @block.sync
            def _(e):
                run("sp", e)

            @block.scalar
            def _(e):
                run("act", e)

            @block.vector
            def _(e):
                run("dve", e)

            @block.gpsimd
            def _(e):
                run("pool", e)

            @block.tensor
            def _(e):
                run("pe", e)


class Ring:
    def __init__(self, nc, name, n, shape, dtype, psum=False):
        self.tiles = []
        for i in range(n):
            nm = "%s_%d" % (name, i)
            t = nc.alloc_psum_tensor(nm, shape, dtype) if psum else nc.alloc_sbuf_tensor(nm, shape, dtype)
            self.tiles.append((nm, t))
        self.i = 0

    def next(self):
        t = self.tiles[self.i % len(self.tiles)]
        self.i += 1
        return t


class Builder:
    def __init__(self, nlayers=NL, dbg=(), mode="full", final=True, parts=("router", "gu", "dn")):
        self.nlayers = nlayers
        self.mode = mode
        self.final = final
        self.parts = parts
        self.nc = nc = bass.Bass("TRN2", target_bir_lowering=False)
        self.S = Sched(nc)
        self.dbg = dbg
        self.inp = {}
        self.scr = {}
        self._alloc()

    def IN(self, name, shape, dt=F32):
        self.inp[name] = self.nc.dram_tensor(name, list(shape), dt, kind="ExternalInput").ap()
        return self.inp[name]

    def SC(self, name, shape, dt=BF16):
        kind = "ExternalOutput" if name in self.dbg else "Internal"
        self.scr[name] = self.nc.dram_tensor(name, list(shape), dt, kind=kind).ap()
        return self.scr[name]

    def _alloc(self):
        nc = self.nc
        L = self.nlayers
        I = self.IN
        mA = self.mode in ("full", "A")
        mB = self.mode in ("full", "B")
        I("hT0", [KC, 128, T])
        I("nfinT", [128, KC])
        I("cmat", [7, 128, 128], BF16)
        I("identF", [128, 128])
        if mA:
            I("cT", [128, KC, 2])
            I("w_ada", [L, D, 6 * D])
            I("b_adaT", [L, 128, 192])
            I("nmixT", [L, 128, KC])
            I("w_in", [L, D, IN_COLS])
            I("qnT", [L, 128, 1])
            I("knT", [L, 128, 1])
            I("mqnT", [L, 128, 8])
            I("mkvnT", [L, 128, 4])
            I("w_uq", [L, 1024, 1536])
            I("w_ukv", [L, 512, 2048])
            I("w_br", [L, D, D])
            I("w_out", [L, D, D])
            I("ropeA", [4, 128, T], BF16)
            I("dftC", [256, 512], BF16)
            I("dftN", [2 * SEQ, SEQ], BF16)
            I("dftX", [2 * NCTX, NCTX], BF16)
        if mB:
            I("nffnT", [L, 128, KC])
            I("w_router", [L, D, NE])
            I("b_routerB", [L, 128, NE])
            if "gu" in self.parts:
                I("w_gu", [L, NE, D, 1024])
            I("b_guT", [L, 128, 256])
            if "dn" in self.parts:
                I("w_dn", [L, NE * 512 + 128, D])
        if self.mode == "B":
            I("modTi", [128, 192, 2])
        if self.mode == "A":
            self.modTo = nc.dram_tensor("modTo", [128, 192, 2], F32, kind="ExternalOutput").ap()
        if self.final and self.mode != "A":
            self.out = nc.dram_tensor("out", [KC, 128, SEQ], F32, kind="ExternalOutput").ap()
        C = self.SC
        C("hT", [KC, 128, T], F32)
        C("uT", [KC, 128, T])
        C("kTa", [4, 128, T])
        C("va", [T, 512])
        C("ckvT", [4, 128, T])
        C("ckvnT", [4, 128, T])
        C("krT", [1, 64, T])
        C("qTa", [16, 128, T])
        C("cqT", [8, 128, T])
        C("cqnT", [8, 128, T])
        C("zfT", [8, 128, T])
        C("gT", [96, 128, T])
        C("kTb", [8, 128, T])
        C("vb", [T, 1024])
        C("qTbn", [8, 128, T])
        C("qTbr", [8, 64, T])
        C("yT", [KC, 128, T])
        C("ABl", [2 * SEQ, 1024])
        C("ABx", [2 * NCTX, 1024])
        C("mT", [KC, 128, T])
        C("combT", [NE, T], F32)
        C("actT", [129, 128, T])
        C("cact", [KC, 128, 2])
        C("junk", [8, 64])
        A = nc.alloc_sbuf_tensor
        self.bres = [A("bres%d" % i, [128, 33 * 512], BF16) for i in range(2)]
        self.ar = Ring(nc, "apc", 3, [128, 33 * 128], BF16)
        self.psA = Ring(nc, "psA", 6, [128, 512], F32, psum=True)
        self.psB = Ring(nc, "psB", 2, [128, 512], F32, psum=True)
        self.f32r = Ring(nc, "f32r", 6, [128, 512], F32)
        self.b16r = Ring(nc, "b16r", 8, [128, 512], BF16)
        self.ldr = Ring(nc, "ldr", 3, [128, 4 * 512], F32)
        self.rope = A("rope", [128, 4, T], BF16)
        self.cm = A("cm", [128, 7, 128], BF16)
        self.identF = A("identFs", [128, 128], F32)
        self.modT = A("modT", [128, 192, 2], F32)
        self.sc = A("scl", [128, 4, KC, 2], F32)
        self.vecs = A("vecs", [128, 4 * KC + 16], F32)
        self.badaT = A("badaTs", [128, 192], F32)
        self.bguT = A("bguTs", [128, 256], F32)
        self.brB = A("brBs", [128, NE], F32)
        self.wrb = A("wrb", [128, KC, NE], BF16)
        self.sm = Ring(nc, "sm", 8, [128, 64], F32)
        self.kres = A("kres", [128, 2, T], BF16)
        self.vres = A("vres", [128, 18, 128], BF16)
        self.qres = Ring(nc, "qres", 2, [128, 2, 512], BF16)
        self.zero = A("zero", [128, 512], BF16)
        self.rstdr = Ring(nc, "rstdr", 2, [128, 512], F32)

    def consts(self):
        S = self.S
        if "ropeA" in self.inp:
            S.dma("sp", self.rope[:], self.inp["ropeA"].rearrange("a p t -> p a t"), writes=["rope"])
        S.dma("sp", self.cm[:], self.inp["cmat"].rearrange("a p t -> p a t"), writes=["cm"])
        S.dma("sp", self.identF[:], self.inp["identF"], writes=["identF"])
        S.add("dve", lambda e: e.memset(self.zero[:], 0.0), writes=["zero"])
        for (t0, n) in TT:
            S.dma("sp", self.scr["actT"][128, :, t0:t0 + n], self.zero[:, 0:n], reads=["zero"], writes=[("actT", 128, t0)])

    def gemm(self, K, mtiles, a_ap, a_f32, a_keys, ntiles, b_ap, b_f32, b_keys, epi, segs=None, group=1, sets=None):
        S = self.S
        nkc = K // 128
        if segs is None:
            segs = [(0, nkc)]
        if sets is None:
            sets = [list(range(len(ntiles)))]
        for st in sets:
            assert len(st) <= 2
            btl = {}
            for si, ti in enumerate(st):
                n = ntiles[ti]
                bt = self.bres[si][:, 0:nkc * n].rearrange("p (k n) -> p k n", n=n)
                S.dma("pool" if b_f32 else "sp", bt, b_ap(ti).rearrange("(k p) n -> p k n", p=128),
                      reads=b_keys(ti), writes=["bres%d" % si])
                btl[ti] = (bt, "bres%d" % si)
            for m0 in range(0, len(mtiles), group):
                grp = list(range(m0, min(m0 + group, len(mtiles))))
                apc = {}
                for mi in grp:
                    m = mtiles[mi]
                    an, at = self.ar.next()
                    av = at[:, 0:nkc * m].rearrange("p (k m) -> p k m", m=m)
                    S.dma("pool" if a_f32 else "sp", av, a_ap(mi).rearrange("(k p) m -> p k m", p=128),
                          reads=a_keys(mi), writes=[an])
                    apc[mi] = (av, an)
                for ti in st:
                    n = ntiles[ti]
                    bt, bk = btl[ti]
                    ps = []
                    for mi in grp:
                        m = mtiles[mi]
                        av, an = apc[mi]
                        for (k0, k1) in segs:
                            pn, pt = self.psA.next()
                            for kc in range(k0, k1):
                                S.add("pe", lambda e, pt=pt, av=av, bt=bt, kc=kc, m=m, n=n, k0=k0, k1=k1:
                                      e.matmul(pt[0:m, 0:n], av[:, kc, :], bt[:, kc, :], start=(kc == k0), stop=(kc == k1 - 1)),
                                      reads=[an, bk], writes=[pn])
                            ps.append((pn, pt))
                    epi(grp, ti, ps)

    def evac_store(self, pn, pt, m, n, dst, dkey, func=None, eng="act"):
        S = self.S
        bn, bt = self.b16r.next()
        if func is None:
            if eng == "act":
                S.add("act", lambda e: e.copy(out=bt[0:m, 0:n], in_=pt[0:m, 0:n]), reads=[pn], writes=[bn])
            else:
                S.add("dve", lambda e: e.tensor_copy(out=bt[0:m, 0:n], in_=pt[0:m, 0:n]), reads=[pn], writes=[bn])
        else:
            S.add("act", lambda e: e.activation(bt[0:m, 0:n], pt[0:m, 0:n], func), reads=[pn], writes=[bn])
        S.dma("sp", dst, bt[0:m, 0:n], reads=[bn], writes=[dkey])

    def rope_store(self, xn, xt, m, n, t0, tab, swap, dst, dkey):
        S = self.S
        cos = self.rope[0:m, tab, t0:t0 + n]
        sin = self.rope[0:m, tab + 1, t0:t0 + n]
        pn, pt = self.psB.next()
        S.add("pe", lambda e: e.matmul(pt[0:m, 0:n], self.cm[0:m, swap, 0:m], xt[0:m, 0:n], start=True, stop=True),
              reads=[xn, "cm"], writes=[pn])
        an, at = self.f32r.next()
        S.add("dve", lambda e: e.tensor_tensor(out=at[0:m, 0:n], in0=xt[0:m, 0:n], in1=cos, op=ALU.mult), reads=[xn, "rope"], writes=[an])
        cn, ct = self.f32r.next()
        S.add("dve", lambda e: e.tensor_tensor(out=ct[0:m, 0:n], in0=pt[0:m, 0:n], in1=sin, op=ALU.mult), reads=[pn, "rope"], writes=[cn])
        on, ot = self.b16r.next()
        S.add("dve", lambda e: e.tensor_tensor(out=ot[0:m, 0:n], in0=at[0:m, 0:n], in1=ct[0:m, 0:n], op=ALU.add), reads=[an, cn], writes=[on])
        S.dma("sp", dst, ot[0:m, 0:n], reads=[on], writes=[dkey])

    def rstd_from_ss(self, pn, pt, n, ring=None):
        S = self.S
        rn, rt = (ring or self.f32r).next()
        S.add("act", lambda e: e.activation(rt[:, 0:n], pt[:, 0:n], AF.Sqrt, bias=EPS, scale=1.0), reads=[pn], writes=[rn])
        S.add("dve", lambda e: e.reciprocal(out=rt[:, 0:n], in_=rt[:, 0:n]), reads=[rn], writes=[rn])
        return rn, rt

    def norm_pass(self, src, skey, nkc, dst, dkey, scale, bias, skeys, ones_idx, dst_f32=False, tiles=None, hook=None, dst_off=0):
        S = self.S
        G = 4
        src_f32 = (src.dtype == F32)
        def body(ti, t0, n):
            pn, pt = self.psB.next()
            for g0 in range(0, nkc, G):
                ln, lt = self.ldr.next()
                gn = min(G, nkc - g0)
                lv = (lt[:, 0:gn * n] if src_f32 else lt[:].bitcast(BF16)[:, 0:gn * n]).rearrange("p (k n) -> p k n", n=n)
                S.dma("sp", lv, src[g0:g0 + gn, :, t0:t0 + n].rearrange("k p n -> p k n"), reads=[(skey, ti)], writes=[ln])
                for k in range(gn):
                    bn, bt = self.b16r.next()
                    S.add("act", lambda e, bt=bt, lv=lv, k=k: e.activation(bt[:, 0:n], lv[:, k, :], AF.Square), reads=[ln], writes=[bn])
                    kc = g0 + k
                    S.add("pe", lambda e, bt=bt, kc=kc, pt=pt: e.matmul(pt[:, 0:n], self.cm[:, ones_idx, :], bt[:, 0:n], start=(kc == 0), stop=(kc == nkc - 1)),
                          reads=[bn, "cm"], writes=[pn])
            rn, rt = self.rstd_from_ss(pn, pt, n, ring=self.rstdr)
            for g0 in range(0, nkc, G):
                ln, lt = self.ldr.next()
                gn = min(G, nkc - g0)
                lv = (lt[:, 0:gn * n] if src_f32 else lt[:].bitcast(BF16)[:, 0:gn * n]).rearrange("p (k n) -> p k n", n=n)
                S.dma("sp", lv, src[g0:g0 + gn, :, t0:t0 + n].rearrange("k p n -> p k n"), reads=[(skey, ti)], writes=[ln])
                for k in range(gn):
                    kc = g0 + k
                    if dst_f32:
                        on, ot = self.f32r.next()
                    else:
                        on, ot = self.b16r.next()
                    if bias is None:
                        S.add("dve", lambda e, ot=ot, lv=lv, k=k, kc=kc: e.scalar_tensor_tensor(out=ot[:, 0:n], in0=lv[:, k, :], scalar=scale(ti, kc), in1=rt[:, 0:n], op0=ALU.mult, op1=ALU.mult),
                              reads=[ln, rn] + skeys, writes=[on])
                    else:
                        xn, xt = self.f32r.next()
                        S.add("dve", lambda e, xt=xt, lv=lv, k=k, kc=kc: e.scalar_tensor_tensor(out=xt[:, 0:n], in0=lv[:, k, :], scalar=scale(ti, kc), in1=rt[:, 0:n], op0=ALU.mult, op1=ALU.mult),
                              reads=[ln, rn] + skeys, writes=[xn])
                        S.add("act", lambda e, ot=ot, xt=xt, kc=kc: e.activation(ot[:, 0:n], xt[:, 0:n], AF.Identity, bias=bias(ti, kc), scale=1.0),
                              reads=[xn] + skeys, writes=[on])
                    if hook is not None:
                        hook(ti, kc, on, ot, n)
                    S.dma("sp", dst[kc, :, t0 - dst_off:t0 - dst_off + n], ot[:, 0:n], reads=[on], writes=[(dkey, ti)])
            if hook is not None:
                hook(ti, None, None, None, n)

        for ti, (t0, n) in enumerate(TT):
            if tiles is not None and ti not in tiles:
                continue
            body(ti, t0, n)

    def load_vecs(self, l):
        S = self.S
        inp = self.inp
        U = ["modT_users"]
        if "b_adaT" in inp:
            S.dma("sp", self.badaT[:], inp["b_adaT"][l], reads=U, writes=["badaT"])
            S.dma("sp", self.vecs[:, 0:KC], inp["nmixT"][l], reads=U, writes=["vecs"])
            S.dma("sp", self.vecs[:, 2 * KC:2 * KC + 1], inp["qnT"][l], reads=U, writes=["vecs"])
            S.dma("sp", self.vecs[:, 2 * KC + 1:2 * KC + 2], inp["knT"][l], reads=U, writes=["vecs"])
            S.dma("sp", self.vecs[:, 2 * KC + 2:2 * KC + 10], inp["mqnT"][l], reads=U, writes=["vecs"])
            S.dma("sp", self.vecs[:, 2 * KC + 10:2 * KC + 14], inp["mkvnT"][l], reads=U, writes=["vecs"])
        if "nffnT" in inp:
            S.dma("sp", self.vecs[:, KC:2 * KC], inp["nffnT"][l], reads=U, writes=["vecs"])
            S.dma("sp", self.bguT[:], inp["b_guT"][l], reads=U, writes=["bguT"])
            S.dma("sp", self.brB[:], inp["b_routerB"][l], reads=U, writes=["brB"])
            S.dma("pool", self.wrb[:], inp["w_router"][l].rearrange("(k p) e -> p k e", p=128), reads=U, writes=["wrb"])
        if l == 0:
            S.dma("sp", self.vecs[:, 3 * KC:4 * KC], inp["nfinT"], writes=["vecsF"])

    def derive(self):
        S = self.S
        for r in range(2):
            if "nmixT" in self.inp:
                S.add("dve", lambda e, r=r: e.scalar_tensor_tensor(out=self.sc[:, 0, :, r], in0=self.modT[:, 32:64, r], scalar=1.0, in1=self.vecs[:, 0:KC], op0=ALU.add, op1=ALU.mult),
                      reads=["modT", "vecs", "modT_users"], writes=["scl"])
            if "nffnT" in self.inp:
                S.add("dve", lambda e, r=r: e.scalar_tensor_tensor(out=self.sc[:, 1, :, r], in0=self.modT[:, 128:160, r], scalar=1.0, in1=self.vecs[:, KC:2 * KC], op0=ALU.add, op1=ALU.mult),
                      reads=["modT", "vecs", "modT_users"], writes=["scl"])

    def mod_phase(self, l):
        S = self.S
        inp = self.inp
        if l == 0:
            cn, ct = self.ldr.next()
            cv = ct[:, 0:KC * 2].rearrange("p (k r) -> p k r", r=2)
            S.dma("sp", cv, inp["cT"], writes=[cn])
            bn, bt = self.b16r.next()
            bv = bt[:, 0:KC * 2].rearrange("p (k r) -> p k r", r=2)
            S.add("act", lambda e: e.activation(bv, cv, AF.Silu), reads=[cn], writes=[bn])
            S.dma("sp", self.scr["cact"].rearrange("k p r -> p k r"), bv, reads=[bn], writes=["cact"])
        self.load_vecs(l)

        def epi(grp, ti, ps):
            mi = grp[0]
            pn, pt = ps[0]
            S.add("dve", lambda e: e.tensor_scalar(self.modT[:, mi, :], pt[:, 0:2], self.badaT[:, mi:mi + 1], None, ALU.add),
                  reads=[pn, "badaT", "modT_users"], writes=["modT"])

        self.gemm(D, [128] * 192, lambda mi: inp["w_ada"][l][:, mi * 128:(mi + 1) * 128], True, lambda mi: [],
                  [2], lambda ti: self.scr["cact"].rearrange("k p r -> (k p) r"), False, lambda ti: ["cact"], epi)
        self.derive()

    MK = ["modT", "scl", "vecs"]

    def mvec(self, j, kc, ti):
        r = 1 if ti == 0 else 0
        return self.modT[:, j * 32 + kc, r:r + 1]

    def inproj(self, l):
        S = self.S
        w = self.inp["w_in"][l]
        scr = self.scr
        mt = []
        for i in range(4):
            mt.append((i * 128, 128, "ka", i))
        for i in range(4):
            mt.append((1024 + i * 128, 128, "ckv", i))
        mt.append((1536, 64, "kr", 0))
        for i in range(16):
            mt.append((1600 + i * 128, 128, "qa", i))
        for i in range(8):
            mt.append((3648 + i * 128, 128, "cq", i))
        for i in range(8):
            mt.append((4672 + i * 128, 128, "f", i))
        for i in range(96):
            mt.append((5696 + i * 128, 128, "g", i))
        ntl = [n for (_, n) in TT]

        def epi(grp, ti, ps):
            mi = grp[0]
            c0, m, kind, i = mt[mi]
            pn, pt = ps[0]
            t0, n = TT[ti]
            if kind in ("qa", "ka"):
                dst = scr["qTa" if kind == "qa" else "kTa"]
                sn, st_ = self.b16r.next()
                S.add("act", lambda e: e.activation(st_[:, 0:n], pt[:, 0:n], AF.Square), reads=[pn], writes=[sn])
                qn, qt = self.psB.next()
                S.add("pe", lambda e: e.matmul(qt[:, 0:n], self.cm[:, 0, :], st_[:, 0:n], start=True, stop=True), reads=[sn, "cm"], writes=[qn])
                rn, rt = self.rstd_from_ss(qn, qt, n)
                xn, xt = self.b16r.next()
                gcol = 2 * KC + (0 if kind == "qa" else 1)
                S.add("dve", lambda e: e.scalar_tensor_tensor(out=xt[:, 0:n], in0=pt[:, 0:n], scalar=self.vecs[:, gcol:gcol + 1], in1=rt[:, 0:n], op0=ALU.mult, op1=ALU.mult),
                      reads=[pn, rn, "vecs"], writes=[xn])
                self.rope_store(xn, xt, 128, n, t0, 0, 1, dst[i, :, t0:t0 + n], (dst.name, i, ti))
            elif kind == "kr":
                xn, xt = self.b16r.next()
                S.add("act", lambda e: e.copy(out=xt[0:64, 0:n], in_=pt[0:64, 0:n]), reads=[pn], writes=[xn])
                self.rope_store(xn, xt, 64, n, t0, 2, 2, scr["krT"][0, :, t0:t0 + n], ("krT", ti))
            elif kind == "g":
                self.evac_store(pn, pt, 128, n, scr["gT"][i, :, t0:t0 + n], ("gT", i, ti), func=AF.Sigmoid)
            else:
                dst = {"ckv": scr["ckvT"], "cq": scr["cqT"], "f": scr["zfT"]}[kind]
                self.evac_store(pn, pt, 128, n, dst[i, :, t0:t0 + n], (dst.name, ti), eng="dve")

        uT2 = scr["uT"].rearrange("k p t -> (k p) t")
        self.gemm(D, [m for (_, m, _, _) in mt], lambda mi: w[:, mt[mi][0]:mt[mi][0] + mt[mi][1]], True, lambda mi: [],
                  ntl, lambda ti: uT2[:, TT[ti][0]:TT[ti][0] + TT[ti][1]], False, lambda ti: [("uT", ti)], epi,
                  sets=[[0, 1], [2, 3], [4]])

        def epi_v(grp, ti, ps):
            mi = grp[0]
            pn, pt = ps[0]
            self.evac_store(pn, pt, 128, 512, scr["va"][mi * 128:(mi + 1) * 128, :], ("va", mi))

        self.gemm(D, [128] * 18, lambda mi: uT2[:, mi * 128:(mi + 1) * 128], False, lambda mi: [("uT", t) for t in range(5)],
                  [512], lambda ti: w[:, 512:1024], True, lambda ti: [], epi_v)

    def mla_up(self, l):
        S = self.S
        scr = self.scr
        ntl = [n for (_, n) in TT]
        self.norm_pass(scr["cqT"], "cqT", 8, scr["cqnT"], "cqnT", lambda ti, kc: self.vecs[:, 2 * KC + 2 + kc:2 * KC + 3 + kc], None, ["vecs"], 5)
        self.norm_pass(scr["ckvT"], "ckvT", 4, scr["ckvnT"], "ckvnT", lambda ti, kc: self.vecs[:, 2 * KC + 10 + kc:2 * KC + 11 + kc], None, ["vecs"], 6)
        wq = self.inp["w_uq"][l]
        wkv = self.inp["w_ukv"][l]
        mt = []
        for h in range(8):
            mt.append((h * 192, 128, "n", h))
            mt.append((h * 192 + 128, 64, "r", h))

        def epi_q(grp, ti, ps):
            mi = grp[0]
            c0, m, kind, h = mt[mi]
            pn, pt = ps[0]
            t0, n = TT[ti]
            if kind == "n":
                self.evac_store(pn, pt, 128, n, scr["qTbn"][h, :, t0:t0 + n], ("qTbn", h, ti))
            else:
                xn, xt = self.b16r.next()
                S.add("act", lambda e: e.copy(out=xt[0:64, 0:n], in_=pt[0:64, 0:n]), reads=[pn], writes=[xn])
                self.rope_store(xn, xt, 64, n, t0, 2, 2, scr["qTbr"][h, :, t0:t0 + n], ("qTbr", h, ti))

        cqn2 = scr["cqnT"].rearrange("k p t -> (k p) t")
        self.gemm(1024, [m for (_, m, _, _) in mt], lambda mi: wq[:, mt[mi][0]:mt[mi][0] + mt[mi][1]], True, lambda mi: [],
                  ntl, lambda ti: cqn2[:, TT[ti][0]:TT[ti][0] + TT[ti][1]], False, lambda ti: [("cqnT", ti)], epi_q,
                  sets=[[0, 1], [2, 3], [4]])

        def epi_k(grp, ti, ps):
            h = grp[0]
            pn, pt = ps[0]
            t0, n = TT[ti]
            self.evac_store(pn, pt, 128, n, scr["kTb"][h, :, t0:t0 + n], ("kTb", h, ti))

        ckvn2 = scr["ckvnT"].rearrange("k p t -> (k p) t")
        self.gemm(512, [128] * 8, lambda h: wkv[:, h * 256:h * 256 + 128], True, lambda mi: [],
                  ntl, lambda ti: ckvn2[:, TT[ti][0]:TT[ti][0] + TT[ti][1]], False, lambda ti: [("ckvnT", ti)], epi_k,
                  sets=[[0, 1], [2, 3], [4]])

        def epi_v(grp, ti, ps):
            mi = grp[0]
            pn, pt = ps[0]
            self.evac_store(pn, pt, 128, 128, scr["vb"][mi * 128:(mi + 1) * 128, ti * 128:(ti + 1) * 128], ("vb", ti, mi))

        self.gemm(512, [128] * 18, lambda mi: ckvn2[:, mi * 128:(mi + 1) * 128], False, lambda mi: [("ckvnT", t) for t in range(5)],
                  [128] * 8, lambda h: wkv[:, h * 256 + 128:h * 256 + 256], True, lambda ti: [], epi_v,
                  sets=[[0, 1], [2, 3], [4, 5], [6, 7]])

    def attention(self, mla):
        S = self.S
        scr = self.scr
        nh = 8 if mla else 16
        scale = MLA_SCALE if mla else GQA_SCALE
        nkv = 8 if mla else 4
        grp = nh // nkv
        for kv in range(nkv):
            if mla:
                S.dma("sp", self.kres[:, 0, :], scr["kTb"][kv], reads=[("kTb", kv, t) for t in range(5)], writes=["kres"])
                S.dma("sp", self.kres[0:64, 1, :], scr["krT"][0], reads=[("krT", t) for t in range(5)], writes=["kres"])
                S.dma("sp", self.vres[:], scr["vb"][:, kv * 128:(kv + 1) * 128].rearrange("(s p) d -> p s d", p=128),
                      reads=[("vb", kv, m) for m in range(18)], writes=["vres"])
            else:
                S.dma("sp", self.kres[:, 0, :], scr["kTa"][kv], reads=[("kTa", kv, t) for t in range(5)], writes=["kres"])
                S.dma("sp", self.vres[:], scr["va"][:, kv * 128:(kv + 1) * 128].rearrange("(s p) d -> p s d", p=128),
                      reads=[("va", m) for m in range(18)], writes=["vres"])
            for hh in range(grp):
                h = kv * grp + hh
                def body(h, ti, t0, n):
                    qn, qt = self.qres.next()
                    if mla:
                        S.dma("sp", qt[:, 0, 0:n], scr["qTbn"][h, :, t0:t0 + n], reads=[("qTbn", h, ti)], writes=[qn])
                        S.dma("sp", qt[0:64, 1, 0:n], scr["qTbr"][h, :, t0:t0 + n], reads=[("qTbr", h, ti)], writes=[qn])
                    else:
                        S.dma("sp", qt[:, 0, 0:n], scr["qTa"][h, :, t0:t0 + n], reads=[("qTa", h, ti)], writes=[qn])
                    nsc = 2 if ti == 0 else 18
                    on_, ot_ = self.psB.next()
                    dn_, dt_ = self.psB.next()
                    for sc in range(nsc):
                        pn, pt = self.psA.next()
                        if mla:
                            S.add("pe", lambda e, pt=pt, qt=qt, sc=sc: e.matmul(pt[:, 0:n], self.kres[:, 0, sc * 128:(sc + 1) * 128], qt[:, 0, 0:n], start=True, stop=False),
                                  reads=["kres", qn], writes=[pn])
                            S.add("pe", lambda e, pt=pt, qt=qt, sc=sc: e.matmul(pt[:, 0:n], self.kres[0:64, 1, sc * 128:(sc + 1) * 128], qt[0:64, 1, 0:n], start=False, stop=True),
                                  reads=["kres", qn], writes=[pn])
                        else:
                            S.add("pe", lambda e, pt=pt, qt=qt, sc=sc: e.matmul(pt[:, 0:n], self.kres[:, 0, sc * 128:(sc + 1) * 128], qt[:, 0, 0:n], start=True, stop=True),
                                  reads=["kres", qn], writes=[pn])
                        en, et = self.b16r.next()
                        S.add("act", lambda e, et=et, pt=pt: e.activation(et[:, 0:n], pt[:, 0:n], AF.Exp, scale=scale), reads=[pn], writes=[en])
                        S.add("pe", lambda e, et=et, sc=sc: e.matmul(ot_[:, 0:n], self.vres[:, sc, :], et[:, 0:n], start=(sc == 0), stop=(sc == nsc - 1)),
                              reads=[en, "vres"], writes=[on_])
                        S.add("pe", lambda e, et=et, sc=sc: e.matmul(dt_[:, 0:n], self.cm[:, 3, :], et[:, 0:n], start=(sc == 0), stop=(sc == nsc - 1)),
                              reads=[en, "cm"], writes=[dn_])
                    rn, rt = self.f32r.next()
                    S.add("dve", lambda e, rt=rt: e.reciprocal(out=rt[:, 0:n], in_=dt_[:, 0:n]), reads=[dn_], writes=[rn])
                    yn, yt = self.b16r.next()
                    S.add("dve", lambda e, rt=rt, yt=yt: e.tensor_tensor(out=yt[:, 0:n], in0=ot_[:, 0:n], in1=rt[:, 0:n], op=ALU.mult), reads=[on_, rn], writes=[yn])
                    yc = (16 + h) if mla else h
                    S.dma("sp", scr["yT"][yc, :, t0:t0 + n], yt[:, 0:n], reads=[yn], writes=[("yT", ti)])

                for ti, (t0, n) in enumerate(TT):
                    body(h, ti, t0, n)

    def fourier(self):
        S = self.S
        scr = self.scr
        inp = self.inp
        zf2 = scr["zfT"].rearrange("k p t -> (k p) t")
        for g in range(4):
            def epi1(grp, ti, ps, g=g):
                mi = grp[0]
                pn, pt = ps[0]
                bn, bt = self.b16r.next()
                S.add("act", lambda e: e.copy(out=bt[:, 0:512], in_=pt[:, 0:512]), reads=[pn], writes=[bn])
                if mi < 2:
                    r0 = mi * 128
                    S.dma("sp", scr["ABx"][r0:r0 + 128, g * 256:(g + 1) * 256], bt[:, 0:256], reads=[bn], writes=[("ABx", g, mi, 0)])
                    S.dma("sp", scr["ABx"][NCTX + r0:NCTX + r0 + 128, g * 256:(g + 1) * 256], bt[:, 256:512], reads=[bn], writes=[("ABx", g, mi, 1)])
                else:
                    r0 = (mi - 2) * 128
                    S.dma("sp", scr["ABl"][r0:r0 + 128, g * 256:(g + 1) * 256], bt[:, 0:256], reads=[bn], writes=[("ABl", g, mi, 0)])
                    S.dma("sp", scr["ABl"][SEQ + r0:SEQ + r0 + 128, g * 256:(g + 1) * 256], bt[:, 256:512], reads=[bn], writes=[("ABl", g, mi, 1)])

            self.gemm(256, [128] * 18, lambda mi, g=g: zf2[g * 256:(g + 1) * 256, mi * 128:(mi + 1) * 128], False,
                      lambda mi: [("zfT", t) for t in range(5)], [512], lambda ti: inp["dftC"], False, lambda ti: [], epi1)

        def epi2(grp, ti, ps):
            mi = grp[0]
            pn, pt = ps[0]
            t0 = NCTX + ti * 512
            self.evac_store(pn, pt, 128, 512, scr["yT"][24 + mi, :, t0:t0 + 512], ("yT", ti + 1))

        abl_keys = [("ABl", g, mi, s) for g in range(4) for mi in range(2, 18) for s in range(2)]
        self.gemm(2 * SEQ, [128] * 8, lambda mi: scr["ABl"][:, mi * 128:(mi + 1) * 128], False, lambda mi: abl_keys,
                  [512] * 4, lambda ti: inp["dftN"][:, ti * 512:(ti + 1) * 512], False, lambda ti: [], epi2, sets=[[0, 1], [2, 3]])

        def epi3(grp, ti, ps):
            mi = grp[0]
            pn, pt = ps[0]
            self.evac_store(pn, pt, 128, 256, scr["yT"][24 + mi, :, 0:256], ("yT", 0))

        abx_keys = [("ABx", g, mi, s) for g in range(4) for mi in range(2) for s in range(2)]
        self.gemm(2 * NCTX, [128] * 8, lambda mi: scr["ABx"][:, mi * 128:(mi + 1) * 128], False, lambda mi: abx_keys,
                  [256], lambda ti: inp["dftX"], False, lambda ti: [], epi3)

    def merge_out(self, l):
        S = self.S
        scr = self.scr
        inp = self.inp
        ntl = [n for (_, n) in TT]
        yT2 = scr["yT"].rearrange("k p t -> (k p) t")

        def epi_m(grp, ti, ps):
            oc = grp[0]
            t0, n = TT[ti]
            acc = None
            for s in range(3):
                pn, pt = ps[s]
                gn, gt = self.b16r.next()
                S.dma("sp", gt[:, 0:n], scr["gT"][s * 32 + oc, :, t0:t0 + n], reads=[("gT", s * 32 + oc, ti)], writes=[gn])
                xn, xt = self.f32r.next()
                S.add("dve", lambda e, xt=xt, pt=pt, gt=gt: e.tensor_tensor(out=xt[:, 0:n], in0=pt[:, 0:n], in1=gt[:, 0:n], op=ALU.mult), reads=[pn, gn], writes=[xn])
                if acc is None:
                    acc = (xn, xt)
                else:
                    an, at = acc
                    if s == 2:
                        on, ot = self.b16r.next()
                    else:
                        on, ot = self.f32r.next()
                    S.add("dve", lambda e, ot=ot, at=at, xt=xt: e.tensor_tensor(out=ot[:, 0:n], in0=at[:, 0:n], in1=xt[:, 0:n], op=ALU.add), reads=[an, xn], writes=[on])
                    acc = (on, ot)
            on, ot = acc
            S.dma("sp", scr["mT"][oc, :, t0:t0 + n], ot[:, 0:n], reads=[on], writes=[("mT", ti)])

        wbr = inp["w_br"][l]
        self.gemm(D, [128] * 32, lambda mi: wbr[:, mi * 128:(mi + 1) * 128], True, lambda mi: [],
                  ntl, lambda ti: yT2[:, TT[ti][0]:TT[ti][0] + TT[ti][1]], False, lambda ti: [("yT", ti)], epi_m,
                  segs=[(0, 16), (16, 24), (24, 32)], sets=[[0, 1], [2, 3], [4]])
        mT2 = scr["mT"].rearrange("k p t -> (k p) t")
        self.gemm(D, [128] * 32, lambda mi: inp["w_out"][l][:, mi * 128:(mi + 1) * 128], True, lambda mi: [],
                  ntl, lambda ti: mT2[:, TT[ti][0]:TT[ti][0] + TT[ti][1]], False, lambda ti: [("mT", ti)],
                  lambda grp, ti, ps: self.resid_epi(grp[0], ti, ps[0], 2), sets=[[0, 1], [2, 3], [4]])

    def resid_epi(self, oc, ti, p, j):
        S = self.S
        pn, pt = p
        t0, n = TT[ti]
        hT = self.scr["hT"]
        hn, ht = self.f32r.next()
        S.dma("sp", ht[:, 0:n], hT[oc, :, t0:t0 + n], reads=[("hT", ti), ("hTw", oc, ti)], writes=[hn])
        on, ot = self.f32r.next()
        S.add("dve", lambda e: e.scalar_tensor_tensor(out=ot[:, 0:n], in0=pt[:, 0:n], scalar=self.mvec(j, oc, ti), in1=ht[:, 0:n], op0=ALU.mult, op1=ALU.add),
              reads=[pn, hn, "modT"], writes=[on])
        S.dma("sp", hT[oc, :, t0:t0 + n], ot[:, 0:n], reads=[on, ("hT", ti)], writes=[("hTw", oc, ti)])

    def h_keys_done(self):
        S = self.S
        for ti in range(5):
            for oc in range(KC):
                pass
            S.dma("sp", self.scr["junk"][ti:ti + 1, :], self.zero[0:1, 0:64], reads=[("hTw", oc, ti) for oc in range(KC)] + ["zero"], writes=[("hT", ti), ("junk", ti)])

    def moe(self, l):
        S = self.S
        scr = self.scr
        inp = self.inp
        ntl = [n for (_, n) in TT]
        rps = {}

        def hook(ti, kc, on, ot, n):
            t0 = TT[ti][0]
            nsb = n // 128
            if kc is not None:
                if kc == 0:
                    rps[ti] = [self.psA.next() for _ in range(nsb)]
                for j in range(nsb):
                    rn, rt = rps[ti][j]
                    S.add("pe", lambda e, j=j, rt=rt, ot=ot, kc=kc: e.matmul(rt[:, 0:32], ot[:, j * 128:(j + 1) * 128], self.wrb[:, kc, :], start=(kc == 0), stop=(kc == KC - 1)),
                          reads=[on, "wrb"], writes=[rn])
                return
            HL = getattr(self, "hook_level", 9)
            for j in range(nsb):
                if HL < 2:
                    break
                rn, rt = rps[ti][j]
                ln, lt = self.sm.next()
                S.add("dve", lambda e, lt=lt, rt=rt: e.tensor_tensor(out=lt[:, 0:32], in0=rt[:, 0:32], in1=self.brB[:], op=ALU.add), reads=[rn, "brB"], writes=[ln])
                S.add("dve", lambda e, lt=lt: e.max(out=lt[:, 32:40], in_=lt[:, 0:32]), reads=[ln], writes=[ln])
                mn, mt_ = self.sm.next()
                S.add("dve", lambda e, lt=lt, mt_=mt_: e.tensor_scalar(mt_[:, 0:32], lt[:, 0:32], lt[:, 35:36], None, ALU.is_ge), reads=[ln], writes=[mn])
                S.add("act", lambda e, lt=lt: e.mul(out=lt[:, 40:41], in_=lt[:, 32:33], mul=-1.0), reads=[ln], writes=[ln])
                S.add("act", lambda e, lt=lt, mt_=mt_: e.activation(mt_[:, 32:64], lt[:, 0:32], AF.Exp, bias=lt[:, 40:41], scale=1.0), reads=[ln, mn], writes=[mn])
                S.add("dve", lambda e, mt_=mt_: e.tensor_tensor(out=mt_[:, 0:32], in0=mt_[:, 0:32], in1=mt_[:, 32:64], op=ALU.mult), reads=[mn], writes=[mn])
                S.add("dve", lambda e, lt=lt, mt_=mt_: e.tensor_reduce(out=lt[:, 41:42], in_=mt_[:, 0:32], axis=mybir.AxisListType.X, op=ALU.add), reads=[mn, ln], writes=[ln])
                S.add("dve", lambda e, lt=lt: e.reciprocal(out=lt[:, 42:43], in_=lt[:, 41:42]), reads=[ln], writes=[ln])
                S.add("dve", lambda e, lt=lt, mt_=mt_: e.tensor_scalar(mt_[:, 0:32], mt_[:, 0:32], lt[:, 42:43], None, ALU.mult), reads=[mn, ln], writes=[mn])
                if HL < 3:
                    continue
                tn, tq = self.sm.next()
                S.add("dve", lambda e, tq=tq, mt_=mt_: e.transpose(out=tq[:, 0:32], in_=mt_[:, 0:32]), reads=[mn], writes=[tn])
                bn, bq = self.b16r.next()
                S.add("act", lambda e, bq=bq, tq=tq: e.copy(out=bq[:, 0:32], in_=tq[:, 0:32]), reads=[tn], writes=[bn])
                if HL < 4:
                    continue
                for g in range(4):
                    c0 = t0 + j * 128 + g * 32
                    S.dma("sp", scr["combT"][:, c0:c0 + 32], tq[32 * g:32 * g + 32, 0:32], reads=[tn], writes=[("combT", ti, j, g)])
                    S.dma("sp", scr["actT"][128, 0:32, c0:c0 + 32], bq[32 * g:32 * g + 32, 0:32], reads=[bn, ("actT", 128, t0)], writes=[("actTc", ti, j, g)])

        self.norm_pass(scr["hT"], "hT", KC, scr["uT"], "uT", lambda ti, kc: self.sc[:, 1, kc, (1 if ti == 0 else 0):(2 if ti == 0 else 1)],
                       lambda ti, kc: self.mvec(3, kc, ti), ["modT", "scl"], 4, hook=(hook if "router" in self.parts else None))
        uT2 = scr["uT"].rearrange("k p t -> (k p) t")
        wgu = inp["w_gu"][l] if "gu" in self.parts else None

        def epi_gu(grp, ti, ps):
            e_ = grp[0] // 8
            jj = (grp[0] % 8) // 2
            t0, n = TT[ti]
            (gn, gp), (un, up) = ps
            cn, ct = self.f32r.next()
            S.dma("sp", ct[:, 0:n], scr["combT"][e_:e_ + 1, t0:t0 + n].to_broadcast([128, n]),
                  reads=[("combT", ti, j, g) for j in range(n // 128) for g in range(4)], writes=[cn])
            an, at = self.f32r.next()
            S.add("dve", lambda e: e.tensor_scalar(at[:, 0:n], gp[:, 0:n], self.bguT[:, grp[0]:grp[0] + 1], 7.0, ALU.add, ALU.min), reads=[gn, "bguT"], writes=[an])
            bn, bt = self.f32r.next()
            S.add("dve", lambda e: e.tensor_scalar(bt[:, 0:n], up[:, 0:n], self.bguT[:, grp[1]:grp[1] + 1], 7.0, ALU.add, ALU.min), reads=[un, "bguT"], writes=[bn])
            S.add("dve", lambda e: e.tensor_scalar(bt[:, 0:n], bt[:, 0:n], -7.0, 1.0, ALU.max, ALU.add), reads=[bn], writes=[bn])
            sn, st_ = self.f32r.next()
            S.add("act", lambda e: e.activation(st_[:, 0:n], at[:, 0:n], AF.Sigmoid, scale=1.702), reads=[an], writes=[sn])
            S.add("dve", lambda e: e.tensor_tensor(out=at[:, 0:n], in0=at[:, 0:n], in1=st_[:, 0:n], op=ALU.mult), reads=[an, sn], writes=[an])
            S.add("dve", lambda e: e.tensor_tensor(out=at[:, 0:n], in0=at[:, 0:n], in1=bt[:, 0:n], op=ALU.mult), reads=[an, bn], writes=[an])
            on, ot = self.b16r.next()
            S.add("dve", lambda e: e.tensor_tensor(out=ot[:, 0:n], in0=at[:, 0:n], in1=ct[:, 0:n], op=ALU.mult), reads=[an, cn], writes=[on])
            S.dma("sp", scr["actT"][e_ * 4 + jj, :, t0:t0 + n], ot[:, 0:n], reads=[on], writes=[("actT", e_ // 8, ti)])

        if "gu" in self.parts:
            self.gemm(D, [128] * 256, lambda mi: wgu[mi // 8][:, (mi % 8) * 128:(mi % 8 + 1) * 128], True, lambda mi: [],
                      ntl, lambda ti: uT2[:, TT[ti][0]:TT[ti][0] + TT[ti][1]], False, lambda ti: [("uT", ti)], epi_gu,
                      group=2, sets=[[0, 1], [2, 3], [4]])
        actT2 = scr["actT"].rearrange("k p t -> (k p) t")
        wdn = inp["w_dn"][l] if "dn" in self.parts else None
        for kg in (range(4) if "dn" in self.parts else ()):
            k0 = kg * 4096
            kk = 4096 + (128 if kg == 3 else 0)

            def bkeys(ti, kg=kg):
                ks = [("actT", kg, ti)]
                if kg == 3:
                    ks += [("actTc", ti, j, g) for j in range(TT[ti][1] // 128) for g in range(4)] + [("actT", 128, TT[ti][0])]
                return ks

            self.gemm(kk, [128] * 32, lambda mi, k0=k0, kk=kk: wdn[k0:k0 + kk, mi * 128:(mi + 1) * 128], True, lambda mi: [],
                      ntl, lambda ti, k0=k0, kk=kk: actT2[k0:k0 + kk, TT[ti][0]:TT[ti][0] + TT[ti][1]], False, bkeys,
                      lambda grp, ti, ps: self.resid_epi(grp[0], ti, ps[0], 5), sets=[[0, 1], [2, 3], [4]])

    def build(self, stop_after=None):
        S = self.S
        scr = self.scr
        self.consts()
        for ti, (t0, n) in enumerate(TT):
            for g0 in range(0, KC, 4):
                ln, lt = self.ldr.next()
                lv = lt[:, 0:4 * n].rearrange("p (k n) -> p k n", n=n)
                S.dma("sp", lv, self.inp["hT0"][g0:g0 + 4, :, t0:t0 + n].rearrange("k p n -> p k n"), writes=[ln])
                S.dma("sp", scr["hT"][g0:g0 + 4, :, t0:t0 + n].rearrange("k p n -> p k n"), lv, reads=[ln], writes=[("hT", ti)])
        for l in range(self.nlayers):
            if self.mode == "B":
                self.load_vecs(l)
                S.dma("sp", self.modT[:], self.inp["modTi"], writes=["modT"])
                self.derive()
            else:
                self.mod_phase(l)
                if self.mode == "A":
                    S.dma("sp", self.modTo, self.modT[:], reads=["modT"])
            if stop_after == "mod":
                break
            if self.mode != "B":
                self.norm_pass(scr["hT"], "hT", KC, scr["uT"], "uT", lambda ti, kc: self.sc[:, 0, kc, (1 if ti == 0 else 0):(2 if ti == 0 else 1)],
                               lambda ti, kc: self.mvec(0, kc, ti), ["modT", "scl"], 4)
                if stop_after == "norm1":
                    break
                self.inproj(l)
                if stop_after == "inproj":
                    break
                self.mla_up(l)
                if stop_after == "mla_up":
                    break
                self.attention(False)
                self.attention(True)
                if stop_after == "attn":
                    break
                self.fourier()
                if stop_after == "fourier":
                    break
                self.merge_out(l)
                self.h_keys_done()
                if stop_after == "mix":
                    break
            if self.mode != "A":
                self.moe(l)
                self.h_keys_done()
                if stop_after == "moe":
                    break
            S.add("dve", lambda e: e.memset(self.sm.tiles[0][1][:, 0:1], 0.0), reads=["modT", "scl", "vecs", "bguT", "brB", "wrb", "badaT"], writes=["modT_users", self.sm.tiles[0][0]])
        if stop_after is None and self.final and self.mode != "A":
            self.norm_pass(scr["hT"], "hT", KC, self.out, "out", lambda ti, kc: self.vecs[:, 3 * KC + kc:3 * KC + kc + 1], None, ["vecsF"], 4,
                           dst_f32=True, tiles=[1, 2, 3, 4], dst_off=NCTX)
        S.emit()
        return self.nc


def _fm(v, n):
    return np.ascontiguousarray(np.asarray(v, np.float32).reshape(n, 128).T)


def _consts():
    theta = 10000.0
    tok = np.arange(SEQ)
    row = (tok // 64).astype(np.float64)
    col = (tok % 64).astype(np.float64)

    def tab(rot):
        half = rot // 2
        q = half // 2
        inv = 1.0 / (theta ** (np.arange(0, half, 2, dtype=np.float64) / half))
        cos = np.ones((rot, T))
        sin = np.zeros((rot, T))
        for d in range(rot):
            pos = row if d < half else col
            dd = d % half
            f = dd % q
            ang = pos * inv[f]
            cos[d, NCTX:] = np.cos(ang)
            sin[d, NCTX:] = np.sin(ang) * (-1.0 if dd < q else 1.0)
        return cos, sin

    ropeA = np.zeros((4, 128, T), np.float32)
    c, s = tab(128)
    ropeA[0], ropeA[1] = c, s
    c, s = tab(64)
    ropeA[2, :64], ropeA[3, :64] = c, s
    cm = np.zeros((7, 128, 128), np.float32)
    cm[0] = 1.0 / 128.0
    for d in range(128):
        cm[1, d, (d + 32) % 64 + 64 * (d // 64)] = 1.0
    for d in range(64):
        cm[2, d, (d + 16) % 32 + 32 * (d // 32)] = 1.0
    cm[3] = 1.0
    cm[4] = 1.0 / 4096.0
    cm[5] = 1.0 / 1024.0
    cm[6] = 1.0 / 512.0
    k = np.arange(256)
    angc = 2 * np.pi * np.outer(k, k) / 256.0
    dftC = np.concatenate([np.cos(angc), -np.sin(angc)], axis=1) / 16.0

    def dn(N):
        kk = np.arange(N)
        a = 2 * np.pi * (np.outer(kk, kk) % N) / N
        return np.concatenate([np.cos(a), np.sin(a)], axis=0) / np.sqrt(N)

    bf = ml_dtypes.bfloat16
    return {
        "ropeA": ropeA.astype(bf), "cmat": cm.astype(bf), "identF": np.eye(128, dtype=np.float32),
        "dftC": dftC.astype(np.float32).astype(bf), "dftN": dn(SEQ).astype(np.float32).astype(bf),
        "dftX": dn(NCTX).astype(np.float32).astype(bf),
    }


def prep_shared(inputs, layers):
    g = {k: np.asarray(v) for k, v in inputs.items()}
    L = len(layers)
    sh = {}
    sh["w_ada"] = np.ascontiguousarray(g["w_ada"][layers])
    sh["b_adaT"] = np.stack([_fm(g["b_ada"][l], 192) for l in layers])
    sh["nmixT"] = np.stack([_fm(g["norm_mix"][l], KC) for l in layers])
    sh["nffnT"] = np.stack([_fm(g["norm_ffn"][l], KC) for l in layers])
    sh["nfinT"] = _fm(g["norm_final"], KC)
    sh["w_in"] = np.ascontiguousarray(g["w_in"][layers])
    sh["qnT"] = np.stack([_fm(g["gqa_q_norm"][l], 1) for l in layers])
    sh["knT"] = np.stack([_fm(g["gqa_k_norm"][l], 1) for l in layers])
    sh["mqnT"] = np.stack([_fm(g["mla_q_norm"][l], 8) for l in layers])
    sh["mkvnT"] = np.stack([_fm(g["mla_kv_norm"][l], 4) for l in layers])
    sh["w_uq"] = np.ascontiguousarray(g["mla_w_uq"][layers])
    sh["w_ukv"] = np.ascontiguousarray(g["mla_w_ukv"][layers])
    sh["w_br"] = np.stack([np.concatenate([g["w_br_gqa"][l], g["w_br_mla"][l], g["w_br_fourier"][l]], axis=0) for l in layers])
    sh["w_out"] = np.ascontiguousarray(g["w_out"][layers])
    sh["w_router"] = np.ascontiguousarray(g["w_router"][layers])
    sh["b_routerB"] = np.stack([np.broadcast_to(g["b_router"][l][None, :], (128, NE)) for l in layers]).astype(np.float32)
    perm = np.concatenate([np.concatenate([2 * np.arange(j * 128, (j + 1) * 128), 2 * np.arange(j * 128, (j + 1) * 128) + 1]) for j in range(4)])
    sh["w_gu"] = np.stack([g["w_gate_up"][l][:, :, perm] for l in layers])
    sh["b_guT"] = np.stack([np.ascontiguousarray(g["b_gate_up"][l][:, perm].reshape(NE * 8, 128).T) for l in layers])
    wd = np.zeros((L, NE * 512 + 128, D), np.float32)
    for i, l in enumerate(layers):
        wd[i, :NE * 512] = g["w_down"][l].reshape(NE * 512, D)
        wd[i, NE * 512:NE * 512 + NE] = g["b_down"][l]
    sh["w_dn"] = wd
    sh.update(_consts())
    return sh


def prep_core(inputs, b):
    x = np.asarray(inputs["x"][b], np.float32)
    ctx = np.asarray(inputs["ctx"][b], np.float32)
    h = np.concatenate([ctx, x], axis=0)
    hT0 = np.ascontiguousarray(h.T).reshape(KC, 128, T)
    c2 = np.stack([np.asarray(inputs["c"][b], np.float32), np.asarray(inputs["c_ctx"], np.float32)], axis=1)
    cT = np.ascontiguousarray(c2.reshape(KC, 128, 2).transpose(1, 0, 2))
    return {"hT0": hT0, "cT": cT}


def kernel(**inputs):
    n = 8
    nc = Builder(NL).build()
    sh = prep_shared(inputs, list(range(NL)))
    in_maps = []
    for b in range(n):
        m = dict(sh)
        m.update(prep_core(inputs, b))
        in_maps.append(m)
    res = run_bass_kernel_spmd(nc, in_maps, core_ids=list(range(n)))
    out = np.empty((n, SEQ, D), np.float32)
    for b in range(n):
        o = res.results[b]["out"]
        out[b] = o.reshape(D, SEQ).T
    return out
```

```python
import numpy as np
import ml_dtypes
import concourse.bass as bass
import concourse.mybir as mybir
from concourse.bass_utils import run_bass_kernel_spmd

F32 = mybir.dt.float32
BF16 = mybir.dt.bfloat16
ALU = mybir.AluOpType
AF = mybir.ActivationFunctionType

D = 4096
KC = 32
NCTX = 256
SEQ = 2048
T = NCTX + SEQ
TT = [(0, 256), (256, 512), (768, 512), (1280, 512), (1792, 512)]
NL = 2
EPS = 1e-6
IN_COLS = 17984
NE = 32
GQA_SCALE = 128 ** -0.5
MLA_SCALE = 192 ** -0.5


def _inproj_tiles():
    mt = []
    for i in range(4):
        mt.append((i * 128, 128, "ka", i))
    for i in range(4):
        mt.append((1024 + i * 128, 128, "ckv", i))
    mt.append((1536, 64, "kr", 0))
    for i in range(16):
        mt.append((1600 + i * 128, 128, "qa", i))
    for i in range(8):
        mt.append((3648 + i * 128, 128, "cq", i))
    for i in range(8):
        mt.append((4672 + i * 128, 128, "f", i))
    for i in range(96):
        mt.append((5696 + i * 128, 128, "g", i))
    return mt


class _Op:
    __slots__ = ("eng", "fn", "deps", "dma", "sem", "target", "need_inc", "prev_slot")

    def __init__(self, eng, fn, dma):
        self.eng = eng
        self.fn = fn
        self.dma = dma
        self.deps = []
        self.sem = None
        self.target = 0
        self.need_inc = False
        self.prev_slot = None


class Sched:
    ENGS = ("pe", "act", "dve", "pool", "sp")
    NDMA = {"sp": 20, "pool": 12, "act": 4}

    def __init__(self, nc):
        self.nc = nc
        self.ops = {e: [] for e in self.ENGS}
        self.lastw = {}
        self.readers = {}

    def add(self, eng, fn, reads=(), writes=(), dma=False):
        op = _Op(eng, fn, dma)
        deps = {}
        for k in reads:
            w = self.lastw.get(k)
            if w is not None:
                deps[id(w)] = w
        for k in writes:
            w = self.lastw.get(k)
            if w is not None:
                deps[id(w)] = w
            for r in self.readers.get(k, {}).values():
                deps[id(r)] = r
        rk = id(op) if dma else eng
        for k in reads:
            self.readers.setdefault(k, {})[rk] = op
        for k in writes:
            self.lastw[k] = op
            self.readers[k] = {}
        for d in deps.values():
            if (not d.dma) and (not dma) and d.eng == "pe" and eng == "pe":
                continue
            op.deps.append(d)
        self.ops[eng].append(op)
        return op

    def dma(self, q, out, in_, reads=(), writes=()):
        return self.add(q, lambda e: e.dma_start(out=out, in_=in_), reads, writes, dma=True)

    def emit(self):
        nc = self.nc
        esem = {e: nc.alloc_semaphore("s_" + e) for e in ("pe", "act", "dve", "pool")}
        dsem = {q: [nc.alloc_semaphore("d_%s%d" % (q, i)) for i in range(n)] for q, n in self.NDMA.items()}
        for e in self.ENGS:
            for op in self.ops[e]:
                for d in op.deps:
                    d.need_inc = True
        for e in self.ENGS:
            cnt = 0
            nd = 0
            slot_last = {}
            for op in self.ops[e]:
                if op.dma:
                    sl = nd % len(dsem[e])
                    nd += 1
                    op.sem = dsem[e][sl]
                    op.prev_slot = slot_last.get(sl)
                    op.target = (op.prev_slot.target if op.prev_slot is not None else 0) + 16
                    slot_last[sl] = op
                else:
                    op.sem = esem[e]
                    if op.need_inc:
                        cnt += 1
                    op.target = cnt
        ops = self.ops

        def run(ename, eng):
            waited = {}
            last = {}
            for op in ops[ename]:
                need = {}
                for d in op.deps:
                    k = id(d.sem)
                    if k not in need or need[k][1] < d.target:
                        need[k] = (d.sem, d.target)
                if op.dma and op.prev_slot is not None:
                    k = id(op.sem)
                    t = op.prev_slot.target
                    if k not in need or need[k][1] < t:
                        need[k] = (op.sem, t)
                for k, (s, t) in need.items():
                    if waited.get(k, 0) >= t:
                        continue
                    eng.wait_ge(s, t)
                    waited[k] = t
                ins = op.fn(eng)
                if op.dma:
                    ins.then_inc(op.sem, 16)
                    last[id(op.sem)] = (op.sem, op.target)
                elif op.need_inc:
                    ins.then_inc(op.sem, 1)
            for k, (s, t) in last.items():
                if waited.get(k, 0) < t:
                    eng.wait_ge(s, t)

        with nc.Block() as block:
            @block.sync
            def _(e):
                run("sp", e)

            @block.scalar
            def _(e):
                run("act", e)

            @block.vector
            def _(e):
                run("dve", e)

            @block.gpsimd
            def _(e):
                run("pool", e)

            @block.tensor
            def _(e):
                run("pe", e)


class Ring:
    def __init__(self, nc, name, n, shape, dtype, psum=False):
        self.tiles = []
        for i in range(n):
            nm = "%s_%d" % (name, i)
            t = nc.alloc_psum_tensor(nm, shape, dtype) if psum else nc.alloc_sbuf_tensor(nm, shape, dtype)
            self.tiles.append((nm, t))
        self.i = 0

    def next(self):
        t = self.tiles[self.i % len(self.tiles)]
        self.i += 1
        return t


class Builder:
    def __init__(self, nlayers=NL, dbg=(), mode="full", final=True, parts=("router", "gu", "dn")):
        self.nlayers = nlayers
        self.mode = mode
        self.final = final
        self.parts = parts
        self.nc = nc = bass.Bass("TRN2", target_bir_lowering=False)
        self.S = Sched(nc)
        self.dbg = dbg
        self.inp = {}
        self.scr = {}
        self._alloc()

    def IN(self, name, shape, dt=F32):
        self.inp[name] = self.nc.dram_tensor(name, list(shape), dt, kind="ExternalInput").ap()
        return self.inp[name]

    def SC(self, name, shape, dt=BF16):
        kind = "ExternalOutput" if name in self.dbg else "Internal"
        self.scr[name] = self.nc.dram_tensor(name, list(shape), dt, kind=kind).ap()
        return self.scr[name]

    def _alloc(self):
        nc = self.nc
        L = self.nlayers
        I = self.IN
        mA = self.mode in ("full", "A")
        mB = self.mode in ("full", "B")
        I("hT0", [KC, 128, T])
        I("nfinT", [128, KC])
        I("cmat", [7, 128, 128], BF16)
        I("identF", [128, 128])
        if mA:
            I("cT", [128, KC, 2])
            I("w_ada", [L, 192, 128, KC, 128])
            I("b_adaT", [L, 128, 192])
            I("nmixT", [L, 128, KC])
            I("w_in", [L, 137, 128, KC, 128])
            I("w_v", [L, D, 512])
            I("qnT", [L, 128, 1])
            I("knT", [L, 128, 1])
            I("mqnT", [L, 128, 8])
            I("mkvnT", [L, 128, 4])
            I("w_uq", [L, 1024, 1536])
            I("w_ukv", [L, 512, 2048])
            I("w_br", [L, KC, 128, KC, 128])
            I("w_out", [L, KC, 128, KC, 128])
            I("ropeA", [4, 128, T], BF16)
            I("dftC", [256, 512], BF16)
            I("dftN", [2 * SEQ, SEQ], BF16)
            I("dftX", [2 * NCTX, NCTX], BF16)
        if mB:
            I("nffnT", [L, 128, KC])
            I("w_router", [L, D, NE])
            I("b_routerB", [L, 128, NE])
            if "gu" in self.parts:
                I("w_gu", [L, NE * 8, 128, KC, 128])
            I("b_guT", [L, 128, 256])
            if "dn" in self.parts:
                I("w_dn", [L, KC, 128, 129, 128])
        if self.mode == "B":
            I("modTi", [128, 192, 2])
        if self.mode == "A":
            self.modTo = nc.dram_tensor("modTo", [128, 192, 2], F32, kind="ExternalOutput").ap()
        if self.final and self.mode != "A":
            self.out = nc.dram_tensor("out", [KC, 128, SEQ], F32, kind="ExternalOutput").ap()
        C = self.SC
        C("hT", [KC, 128, T], F32)
        C("uT", [KC, 128, T])
        C("kTa", [4, 128, T])
        C("va", [T, 512])
        C("ckvT", [4, 128, T])
        C("ckvnT", [4, 128, T])
        C("krT", [1, 64, T])
        C("qTa", [16, 128, T])
        C("cqT", [8, 128, T])
        C("cqnT", [8, 128, T])
        C("zfT", [8, 128, T])
        C("gT", [96, 128, T])
        C("kTb", [8, 128, T])
        C("vb", [T, 1024])
        C("qTbn", [8, 128, T])
        C("qTbr", [8, 64, T])
        C("yT", [KC, 128, T])
        C("ABl", [2 * SEQ, 1024])
        C("ABx", [2 * NCTX, 1024])
        C("mT", [KC, 128, T])
        C("combT", [NE, T], F32)
        C("actT", [129, 128, T])
        C("cact", [KC, 128, 2])
        C("junk", [8, 64])
        A = nc.alloc_sbuf_tensor
        self.bres = [A("bres%d" % i, [128, 33 * 512], BF16) for i in range(2)]
        self.ar = Ring(nc, "apc", 3, [128, 33 * 128], BF16)
        self.psA = Ring(nc, "psA", 6, [128, 512], F32, psum=True)
        self.psB = Ring(nc, "psB", 2, [128, 512], F32, psum=True)
        self.f32r = Ring(nc, "f32r", 6, [128, 512], F32)
        self.b16r = Ring(nc, "b16r", 8, [128, 512], BF16)
        self.ldr = Ring(nc, "ldr", 3, [128, 4 * 512], F32)
        self.rope = A("rope", [128, 4, T], BF16)
        self.cm = A("cm", [128, 7, 128], BF16)
        self.identF = A("identFs", [128, 128], F32)
        self.modT = A("modT", [128, 192, 2], F32)
        self.sc = A("scl", [128, 4, KC, 2], F32)
        self.vecs = A("vecs", [128, 4 * KC + 16], F32)
        self.badaT = A("badaTs", [128, 192], F32)
        self.bguT = A("bguTs", [128, 256], F32)
        self.brB = A("brBs", [128, NE], F32)
        self.wrb = A("wrb", [128, KC, NE], BF16)
        self.sm = Ring(nc, "sm", 8, [128, 64], F32)
        self.kres = A("kres", [128, 2, T], BF16)
        self.vres = A("vres", [128, 18, 128], BF16)
        self.qres = Ring(nc, "qres", 2, [128, 2, 512], BF16)
        self.zero = A("zero", [128, 512], BF16)
        self.rstdr = Ring(nc, "rstdr", 2, [128, 512], F32)

    def consts(self):
        S = self.S
        if "ropeA" in self.inp:
            S.dma("sp", self.rope[:], self.inp["ropeA"].rearrange("a p t -> p a t"), writes=["rope"])
        S.dma("sp", self.cm[:], self.inp["cmat"].rearrange("a p t -> p a t"), writes=["cm"])
        S.dma("sp", self.identF[:], self.inp["identF"], writes=["identF"])
        S.add("dve", lambda e: e.memset(self.zero[:], 0.0), writes=["zero"])
        for (t0, n) in TT:
            S.dma("sp", self.scr["actT"][128, :, t0:t0 + n], self.zero[:, 0:n], reads=["zero"], writes=[("actT", 128, t0)])

    def gemm(self, K, mtiles, a_ap, a_f32, a_keys, ntiles, b_ap, b_f32, b_keys, epi, segs=None, group=1, sets=None, a_pre=False):
        S = self.S
        nkc = K // 128
        if segs is None:
            segs = [(0, nkc)]
        if sets is None:
            sets = [list(range(len(ntiles)))]
        for st in sets:
            assert len(st) <= 2
            btl = {}
            for si, ti in enumerate(st):
                n = ntiles[ti]
                bt = self.bres[si][:, 0:nkc * n].rearrange("p (k n) -> p k n", n=n)
                S.dma("pool" if b_f32 else "sp", bt, b_ap(ti).rearrange("(k p) n -> p k n", p=128),
                      reads=b_keys(ti), writes=["bres%d" % si])
                btl[ti] = (bt, "bres%d" % si)
            for m0 in range(0, len(mtiles), group):
                grp = list(range(m0, min(m0 + group, len(mtiles))))
                apc = {}
                for mi in grp:
                    m = mtiles[mi]
                    an, at = self.ar.next()
                    av = at[:, 0:nkc * m].rearrange("p (k m) -> p k m", m=m)
                    S.dma("pool" if a_f32 else "sp", av, a_ap(mi) if a_pre else a_ap(mi).rearrange("(k p) m -> p k m", p=128),
                          reads=a_keys(mi), writes=[an])
                    apc[mi] = (av, an)
                for ti in st:
                    n = ntiles[ti]
                    bt, bk = btl[ti]
                    ps = []
                    for mi in grp:
                        m = mtiles[mi]
                        av, an = apc[mi]
                        for (k0, k1) in segs:
                            pn, pt = self.psA.next()
                            for kc in range(k0, k1):
                                S.add("pe", lambda e, pt=pt, av=av, bt=bt, kc=kc, m=m, n=n, k0=k0, k1=k1:
                                      e.matmul(pt[0:m, 0:n], av[:, kc, :], bt[:, kc, :], start=(kc == k0), stop=(kc == k1 - 1)),
                                      reads=[an, bk], writes=[pn])
                            ps.append((pn, pt))
                    epi(grp, ti, ps)

    def evac_store(self, pn, pt, m, n, dst, dkey, func=None, eng="act"):
        S = self.S
        bn, bt = self.b16r.next()
        if func is None:
            if eng == "act":
                S.add("act", lambda e: e.copy(out=bt[0:m, 0:n], in_=pt[0:m, 0:n]), reads=[pn], writes=[bn])
            else:
                S.add("dve", lambda e: e.tensor_copy(out=bt[0:m, 0:n], in_=pt[0:m, 0:n]), reads=[pn], writes=[bn])
        else:
            S.add("act", lambda e: e.activation(bt[0:m, 0:n], pt[0:m, 0:n], func), reads=[pn], writes=[bn])
        S.dma("sp", dst, bt[0:m, 0:n], reads=[bn], writes=[dkey])

    def rope_store(self, xn, xt, m, n, t0, tab, swap, dst, dkey):
        S = self.S
        cos = self.rope[0:m, tab, t0:t0 + n]
        sin = self.rope[0:m, tab + 1, t0:t0 + n]
        pn, pt = self.psB.next()
        S.add("pe", lambda e: e.matmul(pt[0:m, 0:n], self.cm[0:m, swap, 0:m], xt[0:m, 0:n], start=True, stop=True),
              reads=[xn, "cm"], writes=[pn])
        an, at = self.f32r.next()
        S.add("dve", lambda e: e.tensor_tensor(out=at[0:m, 0:n], in0=xt[0:m, 0:n], in1=cos, op=ALU.mult), reads=[xn, "rope"], writes=[an])
        cn, ct = self.f32r.next()
        S.add("dve", lambda e: e.tensor_tensor(out=ct[0:m, 0:n], in0=pt[0:m, 0:n], in1=sin, op=ALU.mult), reads=[pn, "rope"], writes=[cn])
        on, ot = self.b16r.next()
        S.add("dve", lambda e: e.tensor_tensor(out=ot[0:m, 0:n], in0=at[0:m, 0:n], in1=ct[0:m, 0:n], op=ALU.add), reads=[an, cn], writes=[on])
        S.dma("sp", dst, ot[0:m, 0:n], reads=[on], writes=[dkey])

    def rstd_from_ss(self, pn, pt, n, ring=None):
        S = self.S
        rn, rt = (ring or self.f32r).next()
        S.add("act", lambda e: e.activation(rt[:, 0:n], pt[:, 0:n], AF.Sqrt, bias=EPS, scale=1.0), reads=[pn], writes=[rn])
        S.add("dve", lambda e: e.reciprocal(out=rt[:, 0:n], in_=rt[:, 0:n]), reads=[rn], writes=[rn])
        return rn, rt

    def norm_pass(self, src, skey, nkc, dst, dkey, scale, bias, skeys, ones_idx, dst_f32=False, tiles=None, hook=None, dst_off=0):
        S = self.S
        G = 4
        src_f32 = (src.dtype == F32)
        def body(ti, t0, n):
            pn, pt = self.psB.next()
            for g0 in range(0, nkc, G):
                ln, lt = self.ldr.next()
                gn = min(G, nkc - g0)
                lv = (lt[:, 0:gn * n] if src_f32 else lt[:].bitcast(BF16)[:, 0:gn * n]).rearrange("p (k n) -> p k n", n=n)
                S.dma("sp", lv, src[g0:g0 + gn, :, t0:t0 + n].rearrange("k p n -> p k n"), reads=[(skey, ti)], writes=[ln])
                for k in range(gn):
                    bn, bt = self.b16r.next()
                    S.add("act", lambda e, bt=bt, lv=lv, k=k: e.activation(bt[:, 0:n], lv[:, k, :], AF.Square), reads=[ln], writes=[bn])
                    kc = g0 + k
                    S.add("pe", lambda e, bt=bt, kc=kc, pt=pt: e.matmul(pt[:, 0:n], self.cm[:, ones_idx, :], bt[:, 0:n], start=(kc == 0), stop=(kc == nkc - 1)),
                          reads=[bn, "cm"], writes=[pn])
            rn, rt = self.rstd_from_ss(pn, pt, n, ring=self.rstdr)
            for g0 in range(0, nkc, G):
                ln, lt = self.ldr.next()
                gn = min(G, nkc - g0)
                lv = (lt[:, 0:gn * n] if src_f32 else lt[:].bitcast(BF16)[:, 0:gn * n]).rearrange("p (k n) -> p k n", n=n)
                S.dma("sp", lv, src[g0:g0 + gn, :, t0:t0 + n].rearrange("k p n -> p k n"), reads=[(skey, ti)], writes=[ln])
                for k in range(gn):
                    kc = g0 + k
                    if dst_f32:
                        on, ot = self.f32r.next()
                    else:
                        on, ot = self.b16r.next()
                    if bias is None:
                        S.add("dve", lambda e, ot=ot, lv=lv, k=k, kc=kc: e.scalar_tensor_tensor(out=ot[:, 0:n], in0=lv[:, k, :], scalar=scale(ti, kc), in1=rt[:, 0:n], op0=ALU.mult, op1=ALU.mult),
                              reads=[ln, rn] + skeys, writes=[on])
                    else:
                        xn, xt = self.f32r.next()
                        S.add("dve", lambda e, xt=xt, lv=lv, k=k, kc=kc: e.scalar_tensor_tensor(out=xt[:, 0:n], in0=lv[:, k, :], scalar=scale(ti, kc), in1=rt[:, 0:n], op0=ALU.mult, op1=ALU.mult),
                              reads=[ln, rn] + skeys, writes=[xn])
                        S.add("act", lambda e, ot=ot, xt=xt, kc=kc: e.activation(ot[:, 0:n], xt[:, 0:n], AF.Identity, bias=bias(ti, kc), scale=1.0),
                              reads=[xn] + skeys, writes=[on])
                    if hook is not None:
                        hook(ti, kc, on, ot, n)
                    S.dma("sp", dst[kc, :, t0 - dst_off:t0 - dst_off + n], ot[:, 0:n], reads=[on], writes=[(dkey, ti)])
            if hook is not None:
                hook(ti, None, None, None, n)

        for ti, (t0, n) in enumerate(TT):
            if tiles is not None and ti not in tiles:
                continue
            body(ti, t0, n)

    def load_vecs(self, l):
        S = self.S
        inp = self.inp
        U = ["modT_users"]
        if "b_adaT" in inp:
            S.dma("sp", self.badaT[:], inp["b_adaT"][l], reads=U, writes=["badaT"])
            S.dma("sp", self.vecs[:, 0:KC], inp["nmixT"][l], reads=U, writes=["vecs"])
            S.dma("sp", self.vecs[:, 2 * KC:2 * KC + 1], inp["qnT"][l], reads=U, writes=["vecs"])
            S.dma("sp", self.vecs[:, 2 * KC + 1:2 * KC + 2], inp["knT"][l], reads=U, writes=["vecs"])
            S.dma("sp", self.vecs[:, 2 * KC + 2:2 * KC + 10], inp["mqnT"][l], reads=U, writes=["vecs"])
            S.dma("sp", self.vecs[:, 2 * KC + 10:2 * KC + 14], inp["mkvnT"][l], reads=U, writes=["vecs"])
        if "nffnT" in inp:
            S.dma("sp", self.vecs[:, KC:2 * KC], inp["nffnT"][l], reads=U, writes=["vecs"])
            S.dma("sp", self.bguT[:], inp["b_guT"][l], reads=U, writes=["bguT"])
            S.dma("sp", self.brB[:], inp["b_routerB"][l], reads=U, writes=["brB"])
            S.dma("pool", self.wrb[:], inp["w_router"][l].rearrange("(k p) e -> p k e", p=128), reads=U, writes=["wrb"])
        if l == 0:
            S.dma("sp", self.vecs[:, 3 * KC:4 * KC], inp["nfinT"], writes=["vecsF"])

    def derive(self):
        S = self.S
        for r in range(2):
            if "nmixT" in self.inp:
                S.add("dve", lambda e, r=r: e.scalar_tensor_tensor(out=self.sc[:, 0, :, r], in0=self.modT[:, 32:64, r], scalar=1.0, in1=self.vecs[:, 0:KC], op0=ALU.add, op1=ALU.mult),
                      reads=["modT", "vecs", "modT_users"], writes=["scl"])
            if "nffnT" in self.inp:
                S.add("dve", lambda e, r=r: e.scalar_tensor_tensor(out=self.sc[:, 1, :, r], in0=self.modT[:, 128:160, r], scalar=1.0, in1=self.vecs[:, KC:2 * KC], op0=ALU.add, op1=ALU.mult),
                      reads=["modT", "vecs", "modT_users"], writes=["scl"])

    def mod_phase(self, l):
        S = self.S
        inp = self.inp
        if l == 0:
            cn, ct = self.ldr.next()
            cv = ct[:, 0:KC * 2].rearrange("p (k r) -> p k r", r=2)
            S.dma("sp", cv, inp["cT"], writes=[cn])
            bn, bt = self.b16r.next()
            bv = bt[:, 0:KC * 2].rearrange("p (k r) -> p k r", r=2)
            S.add("act", lambda e: e.activation(bv, cv, AF.Silu), reads=[cn], writes=[bn])
            S.dma("sp", self.scr["cact"].rearrange("k p r -> p k r"), bv, reads=[bn], writes=["cact"])
        self.load_vecs(l)

        def epi(grp, ti, ps):
            mi = grp[0]
            pn, pt = ps[0]
            S.add("dve", lambda e: e.tensor_scalar(self.modT[:, mi, :], pt[:, 0:2], self.badaT[:, mi:mi + 1], None, ALU.add),
                  reads=[pn, "badaT", "modT_users"], writes=["modT"])

        self.gemm(D, [128] * 192, lambda mi: inp["w_ada"][l][mi], True, lambda mi: [],
                  [2], lambda ti: self.scr["cact"].rearrange("k p r -> (k p) r"), False, lambda ti: ["cact"], epi, a_pre=True)
        self.derive()

    MK = ["modT", "scl", "vecs"]

    def mvec(self, j, kc, ti):
        r = 1 if ti == 0 else 0
        return self.modT[:, j * 32 + kc, r:r + 1]

    def inproj(self, l):
        S = self.S
        w = self.inp["w_in"][l]
        scr = self.scr
        mt = _inproj_tiles()
        ntl = [n for (_, n) in TT]

        def epi(grp, ti, ps):
            mi = grp[0]
            c0, m, kind, i = mt[mi]
            pn, pt = ps[0]
            t0, n = TT[ti]
            if kind in ("qa", "ka"):
                dst = scr["qTa" if kind == "qa" else "kTa"]
                sn, st_ = self.b16r.next()
                S.add("act", lambda e: e.activation(st_[:, 0:n], pt[:, 0:n], AF.Square), reads=[pn], writes=[sn])
                qn, qt = self.psB.next()
                S.add("pe", lambda e: e.matmul(qt[:, 0:n], self.cm[:, 0, :], st_[:, 0:n], start=True, stop=True), reads=[sn, "cm"], writes=[qn])
                rn, rt = self.rstd_from_ss(qn, qt, n)
                xn, xt = self.b16r.next()
                gcol = 2 * KC + (0 if kind == "qa" else 1)
                S.add("dve", lambda e: e.scalar_tensor_tensor(out=xt[:, 0:n], in0=pt[:, 0:n], scalar=self.vecs[:, gcol:gcol + 1], in1=rt[:, 0:n], op0=ALU.mult, op1=ALU.mult),
                      reads=[pn, rn, "vecs"], writes=[xn])
                self.rope_store(xn, xt, 128, n, t0, 0, 1, dst[i, :, t0:t0 + n], (dst.name, i, ti))
            elif kind == "kr":
                xn, xt = self.b16r.next()
                S.add("act", lambda e: e.copy(out=xt[0:64, 0:n], in_=pt[0:64, 0:n]), reads=[pn], writes=[xn])
                self.rope_store(xn, xt, 64, n, t0, 2, 2, scr["krT"][0, :, t0:t0 + n], ("krT", ti))
            elif kind == "g":
                self.evac_store(pn, pt, 128, n, scr["gT"][i, :, t0:t0 + n], ("gT", i, ti), func=AF.Sigmoid)
            else:
                dst = {"ckv": scr["ckvT"], "cq": scr["cqT"], "f": scr["zfT"]}[kind]
                self.evac_store(pn, pt, 128, n, dst[i, :, t0:t0 + n], (dst.name, ti), eng="dve")

        uT2 = scr["uT"].rearrange("k p t -> (k p) t")
        self.gemm(D, [m for (_, m, _, _) in mt], lambda mi: w[mi][:, :, 0:mt[mi][1]], True, lambda mi: [],
                  ntl, lambda ti: uT2[:, TT[ti][0]:TT[ti][0] + TT[ti][1]], False, lambda ti: [("uT", ti)], epi,
                  sets=[[0, 1], [2, 3], [4]], a_pre=True)

        def epi_v(grp, ti, ps):
            mi = grp[0]
            pn, pt = ps[0]
            self.evac_store(pn, pt, 128, 512, scr["va"][mi * 128:(mi + 1) * 128, :], ("va", mi))

        self.gemm(D, [128] * 18, lambda mi: uT2[:, mi * 128:(mi + 1) * 128], False, lambda mi: [("uT", t) for t in range(5)],
                  [512], lambda ti: self.inp["w_v"][l], True, lambda ti: [], epi_v)

    def mla_up(self, l):
        S = self.S
        scr = self.scr
        ntl = [n for (_, n) in TT]
        self.norm_pass(scr["cqT"], "cqT", 8, scr["cqnT"], "cqnT", lambda ti, kc: self.vecs[:, 2 * KC + 2 + kc:2 * KC + 3 + kc], None, ["vecs"], 5)
        self.norm_pass(scr["ckvT"], "ckvT", 4, scr["ckvnT"], "ckvnT", lambda ti, kc: self.vecs[:, 2 * KC + 10 + kc:2 * KC + 11 + kc], None, ["vecs"], 6)
        wq = self.inp["w_uq"][l]
        wkv = self.inp["w_ukv"][l]
        mt = []
        for h in range(8):
            mt.append((h * 192, 128, "n", h))
            mt.append((h * 192 + 128, 64, "r", h))

        def epi_q(grp, ti, ps):
            mi = grp[0]
            c0, m, kind, h = mt[mi]
            pn, pt = ps[0]
            t0, n = TT[ti]
            if kind == "n":
                self.evac_store(pn, pt, 128, n, scr["qTbn"][h, :, t0:t0 + n], ("qTbn", h, ti))
            else:
                xn, xt = self.b16r.next()
                S.add("act", lambda e: e.copy(out=xt[0:64, 0:n], in_=pt[0:64, 0:n]), reads=[pn], writes=[xn])
                self.rope_store(xn, xt, 64, n, t0, 2, 2, scr["qTbr"][h, :, t0:t0 + n], ("qTbr", h, ti))

        cqn2 = scr["cqnT"].rearrange("k p t -> (k p) t")
        self.gemm(1024, [m for (_, m, _, _) in mt], lambda mi: wq[:, mt[mi][0]:mt[mi][0] + mt[mi][1]], True, lambda mi: [],
                  ntl, lambda ti: cqn2[:, TT[ti][0]:TT[ti][0] + TT[ti][1]], False, lambda ti: [("cqnT", ti)], epi_q,
                  sets=[[0, 1], [2, 3], [4]])

        def epi_k(grp, ti, ps):
            h = grp[0]
            pn, pt = ps[0]
            t0, n = TT[ti]
            self.evac_store(pn, pt, 128, n, scr["kTb"][h, :, t0:t0 + n], ("kTb", h, ti))

        ckvn2 = scr["ckvnT"].rearrange("k p t -> (k p) t")
        self.gemm(512, [128] * 8, lambda h: wkv[:, h * 256:h * 256 + 128], True, lambda mi: [],
                  ntl, lambda ti: ckvn2[:, TT[ti][0]:TT[ti][0] + TT[ti][1]], False, lambda ti: [("ckvnT", ti)], epi_k,
                  sets=[[0, 1], [2, 3], [4]])

        def epi_v(grp, ti, ps):
            mi = grp[0]
            pn, pt = ps[0]
            self.evac_store(pn, pt, 128, 128, scr["vb"][mi * 128:(mi + 1) * 128, ti * 128:(ti + 1) * 128], ("vb", ti, mi))

        self.gemm(512, [128] * 18, lambda mi: ckvn2[:, mi * 128:(mi + 1) * 128], False, lambda mi: [("ckvnT", t) for t in range(5)],
                  [128] * 8, lambda h: wkv[:, h * 256 + 128:h * 256 + 256], True, lambda ti: [], epi_v,
                  sets=[[0, 1], [2, 3], [4, 5], [6, 7]])

    def attention(self, mla):
        S = self.S
        scr = self.scr
        nh = 8 if mla else 16
        scale = MLA_SCALE if mla else GQA_SCALE
        nkv = 8 if mla else 4
        grp = nh // nkv
        for kv in range(nkv):
            if mla:
                S.dma("sp", self.kres[:, 0, :], scr["kTb"][kv], reads=[("kTb", kv, t) for t in range(5)], writes=["kres"])
                S.dma("sp", self.kres[0:64, 1, :], scr["krT"][0], reads=[("krT", t) for t in range(5)], writes=["kres"])
                S.dma("sp", self.vres[:], scr["vb"][:, kv * 128:(kv + 1) * 128].rearrange("(s p) d -> p s d", p=128),
                      reads=[("vb", kv, m) for m in range(18)], writes=["vres"])
            else:
                S.dma("sp", self.kres[:, 0, :], scr["kTa"][kv], reads=[("kTa", kv, t) for t in range(5)], writes=["kres"])
                S.dma("sp", self.vres[:], scr["va"][:, kv * 128:(kv + 1) * 128].rearrange("(s p) d -> p s d", p=128),
                      reads=[("va", m) for m in range(18)], writes=["vres"])
            for hh in range(grp):
                h = kv * grp + hh
                def body(h, ti, t0, n):
                    qn, qt = self.qres.next()
                    if mla:
                        S.dma("sp", qt[:, 0, 0:n], scr["qTbn"][h, :, t0:t0 + n], reads=[("qTbn", h, ti)], writes=[qn])
                        S.dma("sp", qt[0:64, 1, 0:n], scr["qTbr"][h, :, t0:t0 + n], reads=[("qTbr", h, ti)], writes=[qn])
                    else:
                        S.dma("sp", qt[:, 0, 0:n], scr["qTa"][h, :, t0:t0 + n], reads=[("qTa", h, ti)], writes=[qn])
                    nsc = 2 if ti == 0 else 18
                    on_, ot_ = self.psB.next()
                    dn_, dt_ = self.psB.next()
                    for sc in range(nsc):
                        pn, pt = self.psA.next()
                        if mla:
                            S.add("pe", lambda e, pt=pt, qt=qt, sc=sc: e.matmul(pt[:, 0:n], self.kres[:, 0, sc * 128:(sc + 1) * 128], qt[:, 0, 0:n], start=True, stop=False),
                                  reads=["kres", qn], writes=[pn])
                            S.add("pe", lambda e, pt=pt, qt=qt, sc=sc: e.matmul(pt[:, 0:n], self.kres[0:64, 1, sc * 128:(sc + 1) * 128], qt[0:64, 1, 0:n], start=False, stop=True),
                                  reads=["kres", qn], writes=[pn])
                        else:
                            S.add("pe", lambda e, pt=pt, qt=qt, sc=sc: e.matmul(pt[:, 0:n], self.kres[:, 0, sc * 128:(sc + 1) * 128], qt[:, 0, 0:n], start=True, stop=True),
                                  reads=["kres", qn], writes=[pn])
                        en, et = self.b16r.next()
                        S.add("act", lambda e, et=et, pt=pt: e.activation(et[:, 0:n], pt[:, 0:n], AF.Exp, scale=scale), reads=[pn], writes=[en])
                        S.add("pe", lambda e, et=et, sc=sc: e.matmul(ot_[:, 0:n], self.vres[:, sc, :], et[:, 0:n], start=(sc == 0), stop=(sc == nsc - 1)),
                              reads=[en, "vres"], writes=[on_])
                        S.add("pe", lambda e, et=et, sc=sc: e.matmul(dt_[:, 0:n], self.cm[:, 3, :], et[:, 0:n], start=(sc == 0), stop=(sc == nsc - 1)),
                              reads=[en, "cm"], writes=[dn_])
                    rn, rt = self.f32r.next()
                    S.add("dve", lambda e, rt=rt: e.reciprocal(out=rt[:, 0:n], in_=dt_[:, 0:n]), reads=[dn_], writes=[rn])
                    yn, yt = self.b16r.next()
                    S.add("dve", lambda e, rt=rt, yt=yt: e.tensor_tensor(out=yt[:, 0:n], in0=ot_[:, 0:n], in1=rt[:, 0:n], op=ALU.mult), reads=[on_, rn], writes=[yn])
                    yc = (16 + h) if mla else h
                    S.dma("sp", scr["yT"][yc, :, t0:t0 + n], yt[:, 0:n], reads=[yn], writes=[("yT", ti)])

                for ti, (t0, n) in enumerate(TT):
                    body(h, ti, t0, n)

    def fourier(self):
        S = self.S
        scr = self.scr
        inp = self.inp
        zf2 = scr["zfT"].rearrange("k p t -> (k p) t")
        for g in range(4):
            def epi1(grp, ti, ps, g=g):
                mi = grp[0]
                pn, pt = ps[0]
                bn, bt = self.b16r.next()
                S.add("act", lambda e: e.copy(out=bt[:, 0:512], in_=pt[:, 0:512]), reads=[pn], writes=[bn])
                if mi < 2:
                    r0 = mi * 128
                    S.dma("sp", scr["ABx"][r0:r0 + 128, g * 256:(g + 1) * 256], bt[:, 0:256], reads=[bn], writes=[("ABx", g, mi, 0)])
                    S.dma("sp", scr["ABx"][NCTX + r0:NCTX + r0 + 128, g * 256:(g + 1) * 256], bt[:, 256:512], reads=[bn], writes=[("ABx", g, mi, 1)])
                else:
                    r0 = (mi - 2) * 128
                    S.dma("sp", scr["ABl"][r0:r0 + 128, g * 256:(g + 1) * 256], bt[:, 0:256], reads=[bn], writes=[("ABl", g, mi, 0)])
                    S.dma("sp", scr["ABl"][SEQ + r0:SEQ + r0 + 128, g * 256:(g + 1) * 256], bt[:, 256:512], reads=[bn], writes=[("ABl", g, mi, 1)])

            self.gemm(256, [128] * 18, lambda mi, g=g: zf2[g * 256:(g + 1) * 256, mi * 128:(mi + 1) * 128], False,
                      lambda mi: [("zfT", t) for t in range(5)], [512], lambda ti: inp["dftC"], False, lambda ti: [], epi1)

        def epi2(grp, ti, ps):
            mi = grp[0]
            pn, pt = ps[0]
            t0 = NCTX + ti * 512
            self.evac_store(pn, pt, 128, 512, scr["yT"][24 + mi, :, t0:t0 + 512], ("yT", ti + 1))

        abl_keys = [("ABl", g, mi, s) for g in range(4) for mi in range(2, 18) for s in range(2)]
        self.gemm(2 * SEQ, [128] * 8, lambda mi: scr["ABl"][:, mi * 128:(mi + 1) * 128], False, lambda mi: abl_keys,
                  [512] * 4, lambda ti: inp["dftN"][:, ti * 512:(ti + 1) * 512], False, lambda ti: [], epi2, sets=[[0, 1], [2, 3]])

        def epi3(grp, ti, ps):
            mi = grp[0]
            pn, pt = ps[0]
            self.evac_store(pn, pt, 128, 256, scr["yT"][24 + mi, :, 0:256], ("yT", 0))

        abx_keys = [("ABx", g, mi, s) for g in range(4) for mi in range(2) for s in range(2)]
        self.gemm(2 * NCTX, [128] * 8, lambda mi: scr["ABx"][:, mi * 128:(mi + 1) * 128], False, lambda mi: abx_keys,
                  [256], lambda ti: inp["dftX"], False, lambda ti: [], epi3)

    def merge_out(self, l):
        S = self.S
        scr = self.scr
        inp = self.inp
        ntl = [n for (_, n) in TT]
        yT2 = scr["yT"].rearrange("k p t -> (k p) t")

        def epi_m(grp, ti, ps):
            oc = grp[0]
            t0, n = TT[ti]
            acc = None
            for s in range(3):
                pn, pt = ps[s]
                gn, gt = self.b16r.next()
                S.dma("sp", gt[:, 0:n], scr["gT"][s * 32 + oc, :, t0:t0 + n], reads=[("gT", s * 32 + oc, ti)], writes=[gn])
                xn, xt = self.f32r.next()
                S.add("dve", lambda e, xt=xt, pt=pt, gt=gt: e.tensor_tensor(out=xt[:, 0:n], in0=pt[:, 0:n], in1=gt[:, 0:n], op=ALU.mult), reads=[pn, gn], writes=[xn])
                if acc is None:
                    acc = (xn, xt)
                else:
                    an, at = acc
                    if s == 2:
                        on, ot = self.b16r.next()
                    else:
                        on, ot = self.f32r.next()
                    S.add("dve", lambda e, ot=ot, at=at, xt=xt: e.tensor_tensor(out=ot[:, 0:n], in0=at[:, 0:n], in1=xt[:, 0:n], op=ALU.add), reads=[an, xn], writes=[on])
                    acc = (on, ot)
            on, ot = acc
            S.dma("sp", scr["mT"][oc, :, t0:t0 + n], ot[:, 0:n], reads=[on], writes=[("mT", ti)])

        wbr = inp["w_br"][l]
        self.gemm(D, [128] * 32, lambda mi: wbr[mi], True, lambda mi: [],
                  ntl, lambda ti: yT2[:, TT[ti][0]:TT[ti][0] + TT[ti][1]], False, lambda ti: [("yT", ti)], epi_m,
                  segs=[(0, 16), (16, 24), (24, 32)], sets=[[0, 1], [2, 3], [4]], a_pre=True)
        mT2 = scr["mT"].rearrange("k p t -> (k p) t")
        self.gemm(D, [128] * 32, lambda mi: inp["w_out"][l][mi], True, lambda mi: [],
                  ntl, lambda ti: mT2[:, TT[ti][0]:TT[ti][0] + TT[ti][1]], False, lambda ti: [("mT", ti)],
                  lambda grp, ti, ps: self.resid_epi(grp[0], ti, ps[0], 2), sets=[[0, 1], [2, 3], [4]], a_pre=True)

    def resid_epi(self, oc, ti, p, j):
        S = self.S
        pn, pt = p
        t0, n = TT[ti]
        hT = self.scr["hT"]
        hn, ht = self.f32r.next()
        S.dma("sp", ht[:, 0:n], hT[oc, :, t0:t0 + n], reads=[("hT", ti), ("hTw", oc, ti)], writes=[hn])
        on, ot = self.f32r.next()
        S.add("dve", lambda e: e.scalar_tensor_tensor(out=ot[:, 0:n], in0=pt[:, 0:n], scalar=self.mvec(j, oc, ti), in1=ht[:, 0:n], op0=ALU.mult, op1=ALU.add),
              reads=[pn, hn, "modT"], writes=[on])
        S.dma("sp", hT[oc, :, t0:t0 + n], ot[:, 0:n], reads=[on, ("hT", ti)], writes=[("hTw", oc, ti)])

    def h_keys_done(self):
        S = self.S
        for ti in range(5):
            for oc in range(KC):
                pass
            S.dma("sp", self.scr["junk"][ti:ti + 1, :], self.zero[0:1, 0:64], reads=[("hTw", oc, ti) for oc in range(KC)] + ["zero"], writes=[("hT", ti), ("junk", ti)])

    def moe(self, l):
        S = self.S
        scr = self.scr
        inp = self.inp
        ntl = [n for (_, n) in TT]
        rps = {}

        def hook(ti, kc, on, ot, n):
            t0 = TT[ti][0]
            nsb = n // 128
            if kc is not None:
                if kc == 0:
                    rps[ti] = [self.psA.next() for _ in range(nsb)]
                for j in range(nsb):
                    rn, rt = rps[ti][j]
                    S.add("pe", lambda e, j=j, rt=rt, ot=ot, kc=kc: e.matmul(rt[:, 0:32], ot[:, j * 128:(j + 1) * 128], self.wrb[:, kc, :], start=(kc == 0), stop=(kc == KC - 1)),
                          reads=[on, "wrb"], writes=[rn])
                return
            HL = getattr(self, "hook_level", 9)
            for j in range(nsb):
                if HL < 2:
                    break
                rn, rt = rps[ti][j]
                ln, lt = self.sm.next()
                S.add("dve", lambda e, lt=lt, rt=rt: e.tensor_tensor(out=lt[:, 0:32], in0=rt[:, 0:32], in1=self.brB[:], op=ALU.add), reads=[rn, "brB"], writes=[ln])
                S.add("dve", lambda e, lt=lt: e.max(out=lt[:, 32:40], in_=lt[:, 0:32]), reads=[ln], writes=[ln])
                mn, mt_ = self.sm.next()
                S.add("dve", lambda e, lt=lt, mt_=mt_: e.tensor_scalar(mt_[:, 0:32], lt[:, 0:32], lt[:, 35:36], None, ALU.is_ge), reads=[ln], writes=[mn])
                S.add("act", lambda e, lt=lt: e.mul(out=lt[:, 40:41], in_=lt[:, 32:33], mul=-1.0), reads=[ln], writes=[ln])
                S.add("act", lambda e, lt=lt, mt_=mt_: e.activation(mt_[:, 32:64], lt[:, 0:32], AF.Exp, bias=lt[:, 40:41], scale=1.0), reads=[ln, mn], writes=[mn])
                S.add("dve", lambda e, mt_=mt_: e.tensor_tensor(out=mt_[:, 0:32], in0=mt_[:, 0:32], in1=mt_[:, 32:64], op=ALU.mult), reads=[mn], writes=[mn])
                S.add("dve", lambda e, lt=lt, mt_=mt_: e.tensor_reduce(out=lt[:, 41:42], in_=mt_[:, 0:32], axis=mybir.AxisListType.X, op=ALU.add), reads=[mn, ln], writes=[ln])
                S.add("dve", lambda e, lt=lt: e.reciprocal(out=lt[:, 42:43], in_=lt[:, 41:42]), reads=[ln], writes=[ln])
                S.add("dve", lambda e, lt=lt, mt_=mt_: e.tensor_scalar(mt_[:, 0:32], mt_[:, 0:32], lt[:, 42:43], None, ALU.mult), reads=[mn, ln], writes=[mn])
                if HL < 3:
                    continue
                tn, tq = self.sm.next()
                S.add("dve", lambda e, tq=tq, mt_=mt_: e.transpose(out=tq[:, 0:32], in_=mt_[:, 0:32]), reads=[mn], writes=[tn])
                bn, bq = self.b16r.next()
                S.add("act", lambda e, bq=bq, tq=tq: e.copy(out=bq[:, 0:32], in_=tq[:, 0:32]), reads=[tn], writes=[bn])
                if HL < 4:
                    continue
                for g in range(4):
                    c0 = t0 + j * 128 + g * 32
                    S.dma("sp", scr["combT"][:, c0:c0 + 32], tq[32 * g:32 * g + 32, 0:32], reads=[tn], writes=[("combT", ti, j, g)])
                    S.dma("sp", scr["actT"][128, 0:32, c0:c0 + 32], bq[32 * g:32 * g + 32, 0:32], reads=[bn, ("actT", 128, t0)], writes=[("actTc", ti, j, g)])

        self.norm_pass(scr["hT"], "hT", KC, scr["uT"], "uT", lambda ti, kc: self.sc[:, 1, kc, (1 if ti == 0 else 0):(2 if ti == 0 else 1)],
                       lambda ti, kc: self.mvec(3, kc, ti), ["modT", "scl"], 4, hook=(hook if "router" in self.parts else None))
        uT2 = scr["uT"].rearrange("k p t -> (k p) t")
        wgu = inp["w_gu"][l] if "gu" in self.parts else None

        def epi_gu(grp, ti, ps):
            e_ = grp[0] // 8
            jj = (grp[0] % 8) // 2
            t0, n = TT[ti]
            (gn, gp), (un, up) = ps
            cn, ct = self.f32r.next()
            S.dma("sp", ct[:, 0:n], scr["combT"][e_:e_ + 1, t0:t0 + n].to_broadcast([128, n]),
                  reads=[("combT", ti, j, g) for j in range(n // 128) for g in range(4)], writes=[cn])
            an, at = self.f32r.next()
            S.add("dve", lambda e: e.tensor_scalar(at[:, 0:n], gp[:, 0:n], self.bguT[:, grp[0]:grp[0] + 1], 7.0, ALU.add, ALU.min), reads=[gn, "bguT"], writes=[an])
            bn, bt = self.f32r.next()
            S.add("dve", lambda e: e.tensor_scalar(bt[:, 0:n], up[:, 0:n], self.bguT[:, grp[1]:grp[1] + 1], 7.0, ALU.add, ALU.min), reads=[un, "bguT"], writes=[bn])
            S.add("dve", lambda e: e.tensor_scalar(bt[:, 0:n], bt[:, 0:n], -7.0, 1.0, ALU.max, ALU.add), reads=[bn], writes=[bn])
            sn, st_ = self.f32r.next()
            S.add("act", lambda e: e.activation(st_[:, 0:n], at[:, 0:n], AF.Sigmoid, scale=1.702), reads=[an], writes=[sn])
            S.add("dve", lambda e: e.tensor_tensor(out=at[:, 0:n], in0=at[:, 0:n], in1=st_[:, 0:n], op=ALU.mult), reads=[an, sn], writes=[an])
            S.add("dve", lambda e: e.tensor_tensor(out=at[:, 0:n], in0=at[:, 0:n], in1=bt[:, 0:n], op=ALU.mult), reads=[an, bn], writes=[an])
            on, ot = self.b16r.next()
            S.add("dve", lambda e: e.tensor_tensor(out=ot[:, 0:n], in0=at[:, 0:n], in1=ct[:, 0:n], op=ALU.mult), reads=[an, cn], writes=[on])
            S.dma("sp", scr["actT"][e_ * 4 + jj, :, t0:t0 + n], ot[:, 0:n], reads=[on], writes=[("actT", e_ // 8, ti)])

        if "gu" in self.parts:
            self.gemm(D, [128] * 256, lambda mi: wgu[mi], True, lambda mi: [],
                      ntl, lambda ti: uT2[:, TT[ti][0]:TT[ti][0] + TT[ti][1]], False, lambda ti: [("uT", ti)], epi_gu,
                      group=2, sets=[[0, 1], [2, 3], [4]], a_pre=True)
        actT2 = scr["actT"].rearrange("k p t -> (k p) t")
        wdn = inp["w_dn"][l] if "dn" in self.parts else None
        for kg in (range(4) if "dn" in self.parts else ()):
            k0 = kg * 4096
            kk = 4096 + (128 if kg == 3 else 0)

            def bkeys(ti, kg=kg):
                ks = [("actT", kg, ti)]
                if kg == 3:
                    ks += [("actTc", ti, j, g) for j in range(TT[ti][1] // 128) for g in range(4)] + [("actT", 128, TT[ti][0])]
                return ks

            self.gemm(kk, [128] * 32, lambda mi, k0=k0, kk=kk: wdn[mi][:, k0 // 128:(k0 + kk) // 128, :], True, lambda mi: [],
                      ntl, lambda ti, k0=k0, kk=kk: actT2[k0:k0 + kk, TT[ti][0]:TT[ti][0] + TT[ti][1]], False, bkeys,
                      lambda grp, ti, ps: self.resid_epi(grp[0], ti, ps[0], 5), sets=[[0, 1], [2, 3], [4]], a_pre=True)

    def build(self, stop_after=None):
        S = self.S
        scr = self.scr
        self.consts()
        for ti, (t0, n) in enumerate(TT):
            for g0 in range(0, KC, 4):
                ln, lt = self.ldr.next()
                lv = lt[:, 0:4 * n].rearrange("p (k n) -> p k n", n=n)
                S.dma("sp", lv, self.inp["hT0"][g0:g0 + 4, :, t0:t0 + n].rearrange("k p n -> p k n"), writes=[ln])
                S.dma("sp", scr["hT"][g0:g0 + 4, :, t0:t0 + n].rearrange("k p n -> p k n"), lv, reads=[ln], writes=[("hT", ti)])
        for l in range(self.nlayers):
            if self.mode == "B":
                self.load_vecs(l)
                S.dma("sp", self.modT[:], self.inp["modTi"], writes=["modT"])
                self.derive()
            else:
                self.mod_phase(l)
                if self.mode == "A":
                    S.dma("sp", self.modTo, self.modT[:], reads=["modT"])
            if stop_after == "mod":
                break
            if self.mode != "B":
                self.norm_pass(scr["hT"], "hT", KC, scr["uT"], "uT", lambda ti, kc: self.sc[:, 0, kc, (1 if ti == 0 else 0):(2 if ti == 0 else 1)],
                               lambda ti, kc: self.mvec(0, kc, ti), ["modT", "scl"], 4)
                if stop_after == "norm1":
                    break
                self.inproj(l)
                if stop_after == "inproj":
                    break
                self.mla_up(l)
                if stop_after == "mla_up":
                    break
                self.attention(False)
                self.attention(True)
                if stop_after == "attn":
                    break
                self.fourier()
                if stop_after == "fourier":
                    break
                self.merge_out(l)
                self.h_keys_done()
                if stop_after == "mix":
                    break
            if self.mode != "A":
                self.moe(l)
                self.h_keys_done()
                if stop_after == "moe":
                    break
            S.add("dve", lambda e: e.memset(self.sm.tiles[0][1][:, 0:1], 0.0), reads=["modT", "scl", "vecs", "bguT", "brB", "wrb", "badaT"], writes=["modT_users", self.sm.tiles[0][0]])
        if stop_after is None and self.final and self.mode != "A":
            self.norm_pass(scr["hT"], "hT", KC, self.out, "out", lambda ti, kc: self.vecs[:, 3 * KC + kc:3 * KC + kc + 1], None, ["vecsF"], 4,
                           dst_f32=True, tiles=[1, 2, 3, 4], dst_off=NCTX)
        S.emit()
        return self.nc


def _fm(v, n):
    return np.ascontiguousarray(np.asarray(v, np.float32).reshape(n, 128).T)


def _consts():
    theta = 10000.0
    tok = np.arange(SEQ)
    row = (tok // 64).astype(np.float64)
    col = (tok % 64).astype(np.float64)

    def tab(rot):
        half = rot // 2
        q = half // 2
        inv = 1.0 / (theta ** (np.arange(0, half, 2, dtype=np.float64) / half))
        cos = np.ones((rot, T))
        sin = np.zeros((rot, T))
        for d in range(rot):
            pos = row if d < half else col
            dd = d % half
            f = dd % q
            ang = pos * inv[f]
            cos[d, NCTX:] = np.cos(ang)
            sin[d, NCTX:] = np.sin(ang) * (-1.0 if dd < q else 1.0)
        return cos, sin

    ropeA = np.zeros((4, 128, T), np.float32)
    c, s = tab(128)
    ropeA[0], ropeA[1] = c, s
    c, s = tab(64)
    ropeA[2, :64], ropeA[3, :64] = c, s
    cm = np.zeros((7, 128, 128), np.float32)
    cm[0] = 1.0 / 128.0
    for d in range(128):
        cm[1, d, (d + 32) % 64 + 64 * (d // 64)] = 1.0
    for d in range(64):
        cm[2, d, (d + 16) % 32 + 32 * (d // 32)] = 1.0
    cm[3] = 1.0
    cm[4] = 1.0 / 4096.0
    cm[5] = 1.0 / 1024.0
    cm[6] = 1.0 / 512.0
    k = np.arange(256)
    angc = 2 * np.pi * np.outer(k, k) / 256.0
    dftC = np.concatenate([np.cos(angc), -np.sin(angc)], axis=1) / 16.0

    def dn(N):
        kk = np.arange(N)
        a = 2 * np.pi * (np.outer(kk, kk) % N) / N
        return np.concatenate([np.cos(a), np.sin(a)], axis=0) / np.sqrt(N)

    bf = ml_dtypes.bfloat16
    return {
        "ropeA": ropeA.astype(bf), "cmat": cm.astype(bf), "identF": np.eye(128, dtype=np.float32),
        "dftC": dftC.astype(np.float32).astype(bf), "dftN": dn(SEQ).astype(np.float32).astype(bf),
        "dftX": dn(NCTX).astype(np.float32).astype(bf),
    }


def _pretile(W, nmi=None):
    K_, N_ = W.shape
    return np.ascontiguousarray(W.reshape(K_ // 128, 128, N_ // 128, 128).transpose(2, 1, 0, 3))


def _pretile_cols(W, tiles):
    K_ = W.shape[0]
    out = np.zeros((len(tiles), 128, K_ // 128, 128), np.float32)
    for i, (c0, m, _, _) in enumerate(tiles):
        out[i, :, :, :m] = W[:, c0:c0 + m].reshape(K_ // 128, 128, m).transpose(1, 0, 2)
    return out


def prep_shared(inputs, layers):
    g = {k: np.asarray(v) for k, v in inputs.items()}
    L = len(layers)
    sh = {}
    sh["w_ada"] = np.stack([_pretile(g["w_ada"][l]) for l in layers])
    sh["b_adaT"] = np.stack([_fm(g["b_ada"][l], 192) for l in layers])
    sh["nmixT"] = np.stack([_fm(g["norm_mix"][l], KC) for l in layers])
    sh["nffnT"] = np.stack([_fm(g["norm_ffn"][l], KC) for l in layers])
    sh["nfinT"] = _fm(g["norm_final"], KC)
    mt = _inproj_tiles()
    sh["w_in"] = np.stack([_pretile_cols(g["w_in"][l], mt) for l in layers])
    sh["w_v"] = np.stack([np.ascontiguousarray(g["w_in"][l][:, 512:1024]) for l in layers])
    sh["qnT"] = np.stack([_fm(g["gqa_q_norm"][l], 1) for l in layers])
    sh["knT"] = np.stack([_fm(g["gqa_k_norm"][l], 1) for l in layers])
    sh["mqnT"] = np.stack([_fm(g["mla_q_norm"][l], 8) for l in layers])
    sh["mkvnT"] = np.stack([_fm(g["mla_kv_norm"][l], 4) for l in layers])
    sh["w_uq"] = np.ascontiguousarray(g["mla_w_uq"][layers])
    sh["w_ukv"] = np.ascontiguousarray(g["mla_w_ukv"][layers])
    sh["w_br"] = np.stack([_pretile(np.concatenate([g["w_br_gqa"][l], g["w_br_mla"][l], g["w_br_fourier"][l]], axis=0)) for l in layers])
    sh["w_out"] = np.stack([_pretile(g["w_out"][l]) for l in layers])
    sh["w_router"] = np.ascontiguousarray(g["w_router"][layers])
    sh["b_routerB"] = np.stack([np.broadcast_to(g["b_router"][l][None, :], (128, NE)) for l in layers]).astype(np.float32)
    perm = np.concatenate([np.concatenate([2 * np.arange(j * 128, (j + 1) * 128), 2 * np.arange(j * 128, (j + 1) * 128) + 1]) for j in range(4)])
    sh["w_gu"] = np.stack([np.concatenate([_pretile(g["w_gate_up"][l][e][:, perm]) for e in range(NE)], axis=0) for l in layers])
    sh["b_guT"] = np.stack([np.ascontiguousarray(g["b_gate_up"][l][:, perm].reshape(NE * 8, 128).T) for l in layers])
    wds = []
    for i, l in enumerate(layers):
        wd = np.zeros((NE * 512 + 128, D), np.float32)
        wd[:NE * 512] = g["w_down"][l].reshape(NE * 512, D)
        wd[NE * 512:NE * 512 + NE] = g["b_down"][l]
        wds.append(_pretile(wd))
    sh["w_dn"] = np.stack(wds)
    sh.update(_consts())
    return sh


def prep_core(inputs, b):
    x = np.asarray(inputs["x"][b], np.float32)
    ctx = np.asarray(inputs["ctx"][b], np.float32)
    h = np.concatenate([ctx, x], axis=0)
    hT0 = np.ascontiguousarray(h.T).reshape(KC, 128, T)
    c2 = np.stack([np.asarray(inputs["c"][b], np.float32), np.asarray(inputs["c_ctx"], np.float32)], axis=1)
    cT = np.ascontiguousarray(c2.reshape(KC, 128, 2).transpose(1, 0, 2))
    return {"hT0": hT0, "cT": cT}


def kernel(**inputs):
    n = 8
    nc = Builder(NL).build()
    sh = prep_shared(inputs, list(range(NL)))
    in_maps = []
    for b in range(n):
        m = dict(sh)
        m.update(prep_core(inputs, b))
        in_maps.append(m)
    res = run_bass_kernel_spmd(nc, in_maps, core_ids=list(range(n)))
    out = np.empty((n, SEQ, D), np.float32)
    for b in range(n):
        o = res.results[b]["out"]
        out[b] = o.reshape(D, SEQ).T
    return out
```

```python
import numpy as np
import ml_dtypes
import concourse.bass as bass
import concourse.mybir as mybir
from concourse.bass_utils import run_bass_kernel_spmd

F32 = mybir.dt.float32
BF16 = mybir.dt.bfloat16
ALU = mybir.AluOpType
AF = mybir.ActivationFunctionType

D = 4096
KC = 32
NCTX = 256
SEQ = 2048
T = NCTX + SEQ
TT = [(0, 256), (256, 512), (768, 512), (1280, 512), (1792, 512)]
NL = 2
EPS = 1e-6
IN_COLS = 17984
NE = 32
GQA_SCALE = 128 ** -0.5
MLA_SCALE = 192 ** -0.5


def _inproj_tiles():
    mt = []
    for i in range(4):
        mt.append((i * 128, 128, "ka", i))
    for i in range(4):
        mt.append((1024 + i * 128, 128, "ckv", i))
    mt.append((1536, 64, "kr", 0))
    for i in range(16):
        mt.append((1600 + i * 128, 128, "qa", i))
    for i in range(8):
        mt.append((3648 + i * 128, 128, "cq", i))
    for i in range(8):
        mt.append((4672 + i * 128, 128, "f", i))
    for i in range(96):
        mt.append((5696 + i * 128, 128, "g", i))
    return mt


class _Op:
    __slots__ = ("eng", "fn", "deps", "dma", "sem", "target", "need_inc", "prev_slot")

    def __init__(self, eng, fn, dma):
        self.eng = eng
        self.fn = fn
        self.dma = dma
        self.deps = []
        self.sem = None
        self.target = 0
        self.need_inc = False
        self.prev_slot = None


class Sched:
    ENGS = ("pe", "act", "dve", "pool", "sp")
    NDMA = {"sp": 20, "pool": 12, "act": 4}

    def __init__(self, nc):
        self.nc = nc
        self.ops = {e: [] for e in self.ENGS}
        self.lastw = {}
        self.readers = {}

    def add(self, eng, fn, reads=(), writes=(), dma=False):
        op = _Op(eng, fn, dma)
        deps = {}
        for k in reads:
            w = self.lastw.get(k)
            if w is not None:
                deps[id(w)] = w
        for k in writes:
            w = self.lastw.get(k)
            if w is not None:
                deps[id(w)] = w
            for r in self.readers.get(k, {}).values():
                deps[id(r)] = r
        rk = id(op) if dma else eng
        for k in reads:
            self.readers.setdefault(k, {})[rk] = op
        for k in writes:
            self.lastw[k] = op
            self.readers[k] = {}
        for d in deps.values():
            if (not d.dma) and (not dma) and d.eng == "pe" and eng == "pe":
                continue
            op.deps.append(d)
        self.ops[eng].append(op)
        return op

    def dma(self, q, out, in_, reads=(), writes=()):
        return self.add(q, lambda e: e.dma_start(out=out, in_=in_), reads, writes, dma=True)

    def emit(self):
        nc = self.nc
        esem = {e: nc.alloc_semaphore("s_" + e) for e in ("pe", "act", "dve", "pool")}
        dsem = {q: [nc.alloc_semaphore("d_%s%d" % (q, i)) for i in range(n)] for q, n in self.NDMA.items()}
        for e in self.ENGS:
            for op in self.ops[e]:
                for d in op.deps:
                    d.need_inc = True
        for e in self.ENGS:
            cnt = 0
            nd = 0
            slot_last = {}
            for op in self.ops[e]:
                if op.dma:
                    sl = nd % len(dsem[e])
                    nd += 1
                    op.sem = dsem[e][sl]
                    op.prev_slot = slot_last.get(sl)
                    op.target = (op.prev_slot.target if op.prev_slot is not None else 0) + 16
                    slot_last[sl] = op
                else:
                    op.sem = esem[e]
                    if op.need_inc:
                        cnt += 1
                    op.target = cnt
        ops = self.ops

        def run(ename, eng):
            waited = {}
            last = {}
            for op in ops[ename]:
                need = {}
                for d in op.deps:
                    k = id(d.sem)
                    if k not in need or need[k][1] < d.target:
                        need[k] = (d.sem, d.target)
                if op.dma and op.prev_slot is not None:
                    k = id(op.sem)
                    t = op.prev_slot.target
                    if k not in need or need[k][1] < t:
                        need[k] = (op.sem, t)
                for k, (s, t) in need.items():
                    if waited.get(k, 0) >= t:
                        continue
                    eng.wait_ge(s, t)
                    waited[k] = t
                ins = op.fn(eng)
                if op.dma:
                    ins.then_inc(op.sem, 16)
                    last[id(op.sem)] = (op.sem, op.target)
                elif op.need_inc:
                    ins.then_inc(op.sem, 1)
            for k, (s, t) in last.items():
                if waited.get(k, 0) < t:
                    eng.wait_ge(s, t)

        with nc.Block() as block:
            @block.sync
            def _(e):
                run("sp", e)

            @block.scalar
            def _(e):
                run("act", e)

            @block.vector
            def _(e):
                run("dve", e)

            @block.gpsimd
            def _(e):
                run("pool", e)

            @block.tensor
            def _(e):
                run("pe", e)


class Ring:
    def __init__(self, nc, name, n, shape, dtype, psum=False):
        self.tiles = []
        for i in range(n):
            nm = "%s_%d" % (name, i)
            t = nc.alloc_psum_tensor(nm, shape, dtype) if psum else nc.alloc_sbuf_tensor(nm, shape, dtype)
            self.tiles.append((nm, t))
        self.i = 0

    def next(self):
        t = self.tiles[self.i % len(self.tiles)]
        self.i += 1
        return t


class Builder:
    def __init__(self, nlayers=NL, dbg=(), mode="full", final=True, parts=("router", "gu", "dn")):
        self.nlayers = nlayers
        self.mode = mode
        self.final = final
        self.parts = parts
        self.nc = nc = bass.Bass("TRN2", target_bir_lowering=False)
        self.S = Sched(nc)
        self.dbg = dbg
        self.inp = {}
        self.scr = {}
        self._alloc()

    def IN(self, name, shape, dt=F32):
        self.inp[name] = self.nc.dram_tensor(name, list(shape), dt, kind="ExternalInput").ap()
        return self.inp[name]

    def SC(self, name, shape, dt=BF16):
        kind = "ExternalOutput" if name in self.dbg else "Internal"
        self.scr[name] = self.nc.dram_tensor(name, list(shape), dt, kind=kind).ap()
        return self.scr[name]

    def _alloc(self):
        nc = self.nc
        L = self.nlayers
        I = self.IN
        mA = self.mode in ("full", "A")
        mB = self.mode in ("full", "B")
        I("hT0", [KC, 128, T])
        I("nfinT", [128, KC])
        I("cmat", [7, 128, 128], BF16)
        I("identF", [128, 128])
        if mA:
            I("cT", [128, KC, 2])
            I("w_ada", [L, 192, 128, KC, 128])
            I("b_adaT", [L, 128, 192])
            I("nmixT", [L, 128, KC])
            I("w_in", [L, 137, 128, KC, 128])
            I("w_v", [L, D, 512])
            I("qnT", [L, 128, 1])
            I("knT", [L, 128, 1])
            I("mqnT", [L, 128, 8])
            I("mkvnT", [L, 128, 4])
            I("w_uq", [L, 1024, 1536])
            I("w_ukv", [L, 512, 2048])
            I("w_br", [L, KC, 128, KC, 128])
            I("w_out", [L, KC, 128, KC, 128])
            I("ropeA", [4, 128, T], BF16)
            I("dftC", [256, 512], BF16)
            I("dftN", [2 * SEQ, SEQ], BF16)
            I("dftX", [2 * NCTX, NCTX], BF16)
        if mB:
            I("nffnT", [L, 128, KC])
            I("w_router", [L, D, NE])
            I("b_routerB", [L, 128, NE])
            if "gu" in self.parts:
                I("w_gu", [L, NE * 8, 128, KC, 128])
            I("b_guT", [L, 128, 256])
            if "dn" in self.parts:
                I("w_dn", [L, KC, 128, 129, 128])
        if self.mode == "B":
            I("modTi", [128, 192, 2])
        if self.mode == "A":
            self.modTo = nc.dram_tensor("modTo", [128, 192, 2], F32, kind="ExternalOutput").ap()
        if self.final and self.mode != "A":
            self.out = nc.dram_tensor("out", [KC, 128, SEQ], F32, kind="ExternalOutput").ap()
        C = self.SC
        C("hT", [KC, 128, T], F32)
        C("uT", [KC, 128, T])
        C("kTa", [4, 128, T])
        C("va", [T, 512])
        C("ckvT", [4, 128, T])
        C("ckvnT", [4, 128, T])
        C("krT", [1, 64, T])
        C("qTa", [16, 128, T])
        C("cqT", [8, 128, T])
        C("cqnT", [8, 128, T])
        C("zfT", [8, 128, T])
        C("gT", [96, 128, T])
        C("kTb", [8, 128, T])
        C("vb", [T, 1024])
        C("qTbn", [8, 128, T])
        C("qTbr", [8, 64, T])
        C("yT", [KC, 128, T])
        C("ABl", [2 * SEQ, 1024])
        C("ABx", [2 * NCTX, 1024])
        C("mT", [KC, 128, T])
        C("combT", [NE, T], F32)
        C("actT", [129, 128, T])
        C("cact", [KC, 128, 2])
        C("junk", [8, 64])
        A = nc.alloc_sbuf_tensor
        self.bres = [A("bres%d" % i, [128, 33 * 512], BF16) for i in range(2)]
        self.ar = Ring(nc, "apc", 4, [128, 33 * 128], BF16)
        self.psA = Ring(nc, "psA", 6, [128, 512], F32, psum=True)
        self.psB = Ring(nc, "psB", 2, [128, 512], F32, psum=True)
        self.f32r = Ring(nc, "f32r", 6, [128, 512], F32)
        self.b16r = Ring(nc, "b16r", 7, [128, 512], BF16)
        self.ldr = Ring(nc, "ldr", 2, [128, 4 * 512], F32)
        self.rope = A("rope", [128, 4, T], BF16)
        self.cm = A("cm", [128, 7, 128], BF16)
        self.identF = A("identFs", [128, 128], F32)
        self.modT = A("modT", [128, 192, 2], F32)
        self.sc = A("scl", [128, 4, KC, 2], F32)
        self.vecs = A("vecs", [128, 4 * KC + 16], F32)
        self.badaT = A("badaTs", [128, 192], F32)
        self.bguT = A("bguTs", [128, 256], F32)
        self.brB = A("brBs", [128, NE], F32)
        self.wrb = A("wrb", [128, KC, NE], BF16)
        self.sm = Ring(nc, "sm", 8, [128, 64], F32)
        self.kres = A("kres", [128, 2, T], BF16)
        self.vres = A("vres", [128, 18, 128], BF16)
        self.qres = Ring(nc, "qres", 2, [128, 2, 512], BF16)
        self.zero = A("zero", [128, 512], BF16)
        self.rstdr = Ring(nc, "rstdr", 2, [128, 512], F32)

    def consts(self):
        S = self.S
        if "ropeA" in self.inp:
            S.dma("sp", self.rope[:], self.inp["ropeA"].rearrange("a p t -> p a t"), writes=["rope"])
        S.dma("sp", self.cm[:], self.inp["cmat"].rearrange("a p t -> p a t"), writes=["cm"])
        S.dma("sp", self.identF[:], self.inp["identF"], writes=["identF"])
        S.add("dve", lambda e: e.memset(self.zero[:], 0.0), writes=["zero"])
        for (t0, n) in TT:
            S.dma("sp", self.scr["actT"][128, :, t0:t0 + n], self.zero[:, 0:n], reads=["zero"], writes=[("actT", 128, t0)])

    def gemm(self, K, mtiles, a_ap, a_f32, a_keys, ntiles, b_ap, b_f32, b_keys, epi, segs=None, group=1, sets=None, a_pre=False):
        S = self.S
        nkc = K // 128
        if segs is None:
            segs = [(0, nkc)]
        if sets is None:
            sets = [list(range(len(ntiles)))]
        for st in sets:
            assert len(st) <= 2
            btl = {}
            for si, ti in enumerate(st):
                n = ntiles[ti]
                bt = self.bres[si][:, 0:nkc * n].rearrange("p (k n) -> p k n", n=n)
                S.dma("pool" if b_f32 else "sp", bt, b_ap(ti).rearrange("(k p) n -> p k n", p=128),
                      reads=b_keys(ti), writes=["bres%d" % si])
                btl[ti] = (bt, "bres%d" % si)
            for m0 in range(0, len(mtiles), group):
                grp = list(range(m0, min(m0 + group, len(mtiles))))
                apc = {}
                for mi in grp:
                    m = mtiles[mi]
                    an, at = self.ar.next()
                    av = at[:, 0:nkc * m].rearrange("p (k m) -> p k m", m=m)
                    S.dma("pool" if a_f32 else "sp", av, a_ap(mi) if a_pre else a_ap(mi).rearrange("(k p) m -> p k m", p=128),
                          reads=a_keys(mi), writes=[an])
                    apc[mi] = (av, an)
                for ti in st:
                    n = ntiles[ti]
                    bt, bk = btl[ti]
                    ps = []
                    for mi in grp:
                        m = mtiles[mi]
                        av, an = apc[mi]
                        for (k0, k1) in segs:
                            pn, pt = self.psA.next()
                            for kc in range(k0, k1):
                                S.add("pe", lambda e, pt=pt, av=av, bt=bt, kc=kc, m=m, n=n, k0=k0, k1=k1:
                                      e.matmul(pt[0:m, 0:n], av[:, kc, :], bt[:, kc, :], start=(kc == k0), stop=(kc == k1 - 1)),
                                      reads=[an, bk], writes=[pn])
                            ps.append((pn, pt))
                    epi(grp, ti, ps)

    def evac_store(self, pn, pt, m, n, dst, dkey, func=None, eng="act"):
        S = self.S
        bn, bt = self.b16r.next()
        if func is None:
            if eng == "act":
                S.add("act", lambda e: e.copy(out=bt[0:m, 0:n], in_=pt[0:m, 0:n]), reads=[pn], writes=[bn])
            else:
                S.add("dve", lambda e: e.tensor_copy(out=bt[0:m, 0:n], in_=pt[0:m, 0:n]), reads=[pn], writes=[bn])
        else:
            S.add("act", lambda e: e.activation(bt[0:m, 0:n], pt[0:m, 0:n], func), reads=[pn], writes=[bn])
        S.dma("sp", dst, bt[0:m, 0:n], reads=[bn], writes=[dkey])

    def rope_store(self, xn, xt, m, n, t0, tab, swap, dst, dkey):
        S = self.S
        cos = self.rope[0:m, tab, t0:t0 + n]
        sin = self.rope[0:m, tab + 1, t0:t0 + n]
        pn, pt = self.psB.next()
        S.add("pe", lambda e: e.matmul(pt[0:m, 0:n], self.cm[0:m, swap, 0:m], xt[0:m, 0:n], start=True, stop=True),
              reads=[xn, "cm"], writes=[pn])
        an, at = self.f32r.next()
        S.add("dve", lambda e: e.tensor_tensor(out=at[0:m, 0:n], in0=xt[0:m, 0:n], in1=cos, op=ALU.mult), reads=[xn, "rope"], writes=[an])
        cn, ct = self.f32r.next()
        S.add("dve", lambda e: e.tensor_tensor(out=ct[0:m, 0:n], in0=pt[0:m, 0:n], in1=sin, op=ALU.mult), reads=[pn, "rope"], writes=[cn])
        on, ot = self.b16r.next()
        S.add("dve", lambda e: e.tensor_tensor(out=ot[0:m, 0:n], in0=at[0:m, 0:n], in1=ct[0:m, 0:n], op=ALU.add), reads=[an, cn], writes=[on])
        S.dma("sp", dst, ot[0:m, 0:n], reads=[on], writes=[dkey])

    def rstd_from_ss(self, pn, pt, n, ring=None):
        S = self.S
        rn, rt = (ring or self.f32r).next()
        S.add("act", lambda e: e.activation(rt[:, 0:n], pt[:, 0:n], AF.Sqrt, bias=EPS, scale=1.0), reads=[pn], writes=[rn])
        S.add("dve", lambda e: e.reciprocal(out=rt[:, 0:n], in_=rt[:, 0:n]), reads=[rn], writes=[rn])
        return rn, rt

    def norm_pass(self, src, skey, nkc, dst, dkey, scale, bias, skeys, ones_idx, dst_f32=False, tiles=None, hook=None, dst_off=0):
        S = self.S
        G = 4
        src_f32 = (src.dtype == F32)
        def body(ti, t0, n):
            pn, pt = self.psB.next()
            for g0 in range(0, nkc, G):
                ln, lt = self.ldr.next()
                gn = min(G, nkc - g0)
                lv = (lt[:, 0:gn * n] if src_f32 else lt[:].bitcast(BF16)[:, 0:gn * n]).rearrange("p (k n) -> p k n", n=n)
                S.dma("sp", lv, src[g0:g0 + gn, :, t0:t0 + n].rearrange("k p n -> p k n"), reads=[(skey, ti)], writes=[ln])
                for k in range(gn):
                    bn, bt = self.b16r.next()
                    S.add("act", lambda e, bt=bt, lv=lv, k=k: e.activation(bt[:, 0:n], lv[:, k, :], AF.Square), reads=[ln], writes=[bn])
                    kc = g0 + k
                    S.add("pe", lambda e, bt=bt, kc=kc, pt=pt: e.matmul(pt[:, 0:n], self.cm[:, ones_idx, :], bt[:, 0:n], start=(kc == 0), stop=(kc == nkc - 1)),
                          reads=[bn, "cm"], writes=[pn])
            rn, rt = self.rstd_from_ss(pn, pt, n, ring=self.rstdr)
            for g0 in range(0, nkc, G):
                ln, lt = self.ldr.next()
                gn = min(G, nkc - g0)
                lv = (lt[:, 0:gn * n] if src_f32 else lt[:].bitcast(BF16)[:, 0:gn * n]).rearrange("p (k n) -> p k n", n=n)
                S.dma("sp", lv, src[g0:g0 + gn, :, t0:t0 + n].rearrange("k p n -> p k n"), reads=[(skey, ti)], writes=[ln])
                for k in range(gn):
                    kc = g0 + k
                    if dst_f32:
                        on, ot = self.f32r.next()
                    else:
                        on, ot = self.b16r.next()
                    if bias is None:
                        S.add("dve", lambda e, ot=ot, lv=lv, k=k, kc=kc: e.scalar_tensor_tensor(out=ot[:, 0:n], in0=lv[:, k, :], scalar=scale(ti, kc), in1=rt[:, 0:n], op0=ALU.mult, op1=ALU.mult),
                              reads=[ln, rn] + skeys, writes=[on])
                    else:
                        xn, xt = self.f32r.next()
                        S.add("dve", lambda e, xt=xt, lv=lv, k=k, kc=kc: e.scalar_tensor_tensor(out=xt[:, 0:n], in0=lv[:, k, :], scalar=scale(ti, kc), in1=rt[:, 0:n], op0=ALU.mult, op1=ALU.mult),
                              reads=[ln, rn] + skeys, writes=[xn])
                        S.add("act", lambda e, ot=ot, xt=xt, kc=kc: e.activation(ot[:, 0:n], xt[:, 0:n], AF.Identity, bias=bias(ti, kc), scale=1.0),
                              reads=[xn] + skeys, writes=[on])
                    if hook is not None:
                        hook(ti, kc, on, ot, n)
                    S.dma("sp", dst[kc, :, t0 - dst_off:t0 - dst_off + n], ot[:, 0:n], reads=[on], writes=[(dkey, ti)])
            if hook is not None:
                hook(ti, None, None, None, n)

        for ti, (t0, n) in enumerate(TT):
            if tiles is not None and ti not in tiles:
                continue
            body(ti, t0, n)

    def load_vecs(self, l):
        S = self.S
        inp = self.inp
        U = ["modT_users"]
        if "b_adaT" in inp:
            S.dma("sp", self.badaT[:], inp["b_adaT"][l], reads=U, writes=["badaT"])
            S.dma("sp", self.vecs[:, 0:KC], inp["nmixT"][l], reads=U, writes=["vecs"])
            S.dma("sp", self.vecs[:, 2 * KC:2 * KC + 1], inp["qnT"][l], reads=U, writes=["vecs"])
            S.dma("sp", self.vecs[:, 2 * KC + 1:2 * KC + 2], inp["knT"][l], reads=U, writes=["vecs"])
            S.dma("sp", self.vecs[:, 2 * KC + 2:2 * KC + 10], inp["mqnT"][l], reads=U, writes=["vecs"])
            S.dma("sp", self.vecs[:, 2 * KC + 10:2 * KC + 14], inp["mkvnT"][l], reads=U, writes=["vecs"])
        if "nffnT" in inp:
            S.dma("sp", self.vecs[:, KC:2 * KC], inp["nffnT"][l], reads=U, writes=["vecs"])
            S.dma("sp", self.bguT[:], inp["b_guT"][l], reads=U, writes=["bguT"])
            S.dma("sp", self.brB[:], inp["b_routerB"][l], reads=U, writes=["brB"])
            S.dma("pool", self.wrb[:], inp["w_router"][l].rearrange("(k p) e -> p k e", p=128), reads=U, writes=["wrb"])
        if l == 0:
            S.dma("sp", self.vecs[:, 3 * KC:4 * KC], inp["nfinT"], writes=["vecsF"])

    def derive(self):
        S = self.S
        for r in range(2):
            if "nmixT" in self.inp:
                S.add("dve", lambda e, r=r: e.scalar_tensor_tensor(out=self.sc[:, 0, :, r], in0=self.modT[:, 32:64, r], scalar=1.0, in1=self.vecs[:, 0:KC], op0=ALU.add, op1=ALU.mult),
                      reads=["modT", "vecs", "modT_users"], writes=["scl"])
            if "nffnT" in self.inp:
                S.add("dve", lambda e, r=r: e.scalar_tensor_tensor(out=self.sc[:, 1, :, r], in0=self.modT[:, 128:160, r], scalar=1.0, in1=self.vecs[:, KC:2 * KC], op0=ALU.add, op1=ALU.mult),
                      reads=["modT", "vecs", "modT_users"], writes=["scl"])

    def mod_phase(self, l):
        S = self.S
        inp = self.inp
        if l == 0:
            cn, ct = self.ldr.next()
            cv = ct[:, 0:KC * 2].rearrange("p (k r) -> p k r", r=2)
            S.dma("sp", cv, inp["cT"], writes=[cn])
            bn, bt = self.b16r.next()
            bv = bt[:, 0:KC * 2].rearrange("p (k r) -> p k r", r=2)
            S.add("act", lambda e: e.activation(bv, cv, AF.Silu), reads=[cn], writes=[bn])
            S.dma("sp", self.scr["cact"].rearrange("k p r -> p k r"), bv, reads=[bn], writes=["cact"])
        self.load_vecs(l)

        def epi(grp, ti, ps):
            mi = grp[0]
            pn, pt = ps[0]
            S.add("dve", lambda e: e.tensor_scalar(self.modT[:, mi, :], pt[:, 0:2], self.badaT[:, mi:mi + 1], None, ALU.add),
                  reads=[pn, "badaT", "modT_users"], writes=["modT"])

        self.gemm(D, [128] * 192, lambda mi: inp["w_ada"][l][mi], True, lambda mi: [],
                  [2], lambda ti: self.scr["cact"].rearrange("k p r -> (k p) r"), False, lambda ti: ["cact"], epi, a_pre=True)
        self.derive()

    MK = ["modT", "scl", "vecs"]

    def mvec(self, j, kc, ti):
        r = 1 if ti == 0 else 0
        return self.modT[:, j * 32 + kc, r:r + 1]

    def inproj(self, l):
        S = self.S
        w = self.inp["w_in"][l]
        scr = self.scr
        mt = _inproj_tiles()
        ntl = [n for (_, n) in TT]

        def epi(grp, ti, ps):
            mi = grp[0]
            c0, m, kind, i = mt[mi]
            pn, pt = ps[0]
            t0, n = TT[ti]
            if kind in ("qa", "ka"):
                dst = scr["qTa" if kind == "qa" else "kTa"]
                sn, st_ = self.b16r.next()
                S.add("act", lambda e: e.activation(st_[:, 0:n], pt[:, 0:n], AF.Square), reads=[pn], writes=[sn])
                qn, qt = self.psB.next()
                S.add("pe", lambda e: e.matmul(qt[:, 0:n], self.cm[:, 0, :], st_[:, 0:n], start=True, stop=True), reads=[sn, "cm"], writes=[qn])
                rn, rt = self.rstd_from_ss(qn, qt, n)
                xn, xt = self.b16r.next()
                gcol = 2 * KC + (0 if kind == "qa" else 1)
                S.add("dve", lambda e: e.scalar_tensor_tensor(out=xt[:, 0:n], in0=pt[:, 0:n], scalar=self.vecs[:, gcol:gcol + 1], in1=rt[:, 0:n], op0=ALU.mult, op1=ALU.mult),
                      reads=[pn, rn, "vecs"], writes=[xn])
                self.rope_store(xn, xt, 128, n, t0, 0, 1, dst[i, :, t0:t0 + n], (dst.name, i, ti))
            elif kind == "kr":
                xn, xt = self.b16r.next()
                S.add("act", lambda e: e.copy(out=xt[0:64, 0:n], in_=pt[0:64, 0:n]), reads=[pn], writes=[xn])
                self.rope_store(xn, xt, 64, n, t0, 2, 2, scr["krT"][0, :, t0:t0 + n], ("krT", ti))
            elif kind == "g":
                self.evac_store(pn, pt, 128, n, scr["gT"][i, :, t0:t0 + n], ("gT", i, ti), func=AF.Sigmoid)
            else:
                dst = {"ckv": scr["ckvT"], "cq": scr["cqT"], "f": scr["zfT"]}[kind]
                self.evac_store(pn, pt, 128, n, dst[i, :, t0:t0 + n], (dst.name, ti), eng="dve")

        uT2 = scr["uT"].rearrange("k p t -> (k p) t")
        self.gemm(D, [m for (_, m, _, _) in mt], lambda mi: w[mi][:, :, 0:mt[mi][1]], True, lambda mi: [],
                  ntl, lambda ti: uT2[:, TT[ti][0]:TT[ti][0] + TT[ti][1]], False, lambda ti: [("uT", ti)], epi,
                  sets=[[0, 1], [2, 3], [4]], a_pre=True)

        def epi_v(grp, ti, ps):
            mi = grp[0]
            pn, pt = ps[0]
            self.evac_store(pn, pt, 128, 512, scr["va"][mi * 128:(mi + 1) * 128, :], ("va", mi))

        self.gemm(D, [128] * 18, lambda mi: uT2[:, mi * 128:(mi + 1) * 128], False, lambda mi: [("uT", t) for t in range(5)],
                  [512], lambda ti: self.inp["w_v"][l], True, lambda ti: [], epi_v)

    def mla_up(self, l):
        S = self.S
        scr = self.scr
        ntl = [n for (_, n) in TT]
        self.norm_pass(scr["cqT"], "cqT", 8, scr["cqnT"], "cqnT", lambda ti, kc: self.vecs[:, 2 * KC + 2 + kc:2 * KC + 3 + kc], None, ["vecs"], 5)
        self.norm_pass(scr["ckvT"], "ckvT", 4, scr["ckvnT"], "ckvnT", lambda ti, kc: self.vecs[:, 2 * KC + 10 + kc:2 * KC + 11 + kc], None, ["vecs"], 6)
        wq = self.inp["w_uq"][l]
        wkv = self.inp["w_ukv"][l]
        mt = []
        for h in range(8):
            mt.append((h * 192, 128, "n", h))
            mt.append((h * 192 + 128, 64, "r", h))

        def epi_q(grp, ti, ps):
            mi = grp[0]
            c0, m, kind, h = mt[mi]
            pn, pt = ps[0]
            t0, n = TT[ti]
            if kind == "n":
                self.evac_store(pn, pt, 128, n, scr["qTbn"][h, :, t0:t0 + n], ("qTbn", h, ti))
            else:
                xn, xt = self.b16r.next()
                S.add("act", lambda e: e.copy(out=xt[0:64, 0:n], in_=pt[0:64, 0:n]), reads=[pn], writes=[xn])
                self.rope_store(xn, xt, 64, n, t0, 2, 2, scr["qTbr"][h, :, t0:t0 + n], ("qTbr", h, ti))

        cqn2 = scr["cqnT"].rearrange("k p t -> (k p) t")
        self.gemm(1024, [m for (_, m, _, _) in mt], lambda mi: wq[:, mt[mi][0]:mt[mi][0] + mt[mi][1]], True, lambda mi: [],
                  ntl, lambda ti: cqn2[:, TT[ti][0]:TT[ti][0] + TT[ti][1]], False, lambda ti: [("cqnT", ti)], epi_q,
                  sets=[[0, 1], [2, 3], [4]])

        def epi_k(grp, ti, ps):
            h = grp[0]
            pn, pt = ps[0]
            t0, n = TT[ti]
            self.evac_store(pn, pt, 128, n, scr["kTb"][h, :, t0:t0 + n], ("kTb", h, ti))

        ckvn2 = scr["ckvnT"].rearrange("k p t -> (k p) t")
        self.gemm(512, [128] * 8, lambda h: wkv[:, h * 256:h * 256 + 128], True, lambda mi: [],
                  ntl, lambda ti: ckvn2[:, TT[ti][0]:TT[ti][0] + TT[ti][1]], False, lambda ti: [("ckvnT", ti)], epi_k,
                  sets=[[0, 1], [2, 3], [4]])

        def epi_v(grp, ti, ps):
            mi = grp[0]
            pn, pt = ps[0]
            self.evac_store(pn, pt, 128, 128, scr["vb"][mi * 128:(mi + 1) * 128, ti * 128:(ti + 1) * 128], ("vb", ti, mi))

        self.gemm(512, [128] * 18, lambda mi: ckvn2[:, mi * 128:(mi + 1) * 128], False, lambda mi: [("ckvnT", t) for t in range(5)],
                  [128] * 8, lambda h: wkv[:, h * 256 + 128:h * 256 + 256], True, lambda ti: [], epi_v,
                  sets=[[0, 1], [2, 3], [4, 5], [6, 7]])

    def attention(self, mla):
        S = self.S
        scr = self.scr
        nh = 8 if mla else 16
        scale = MLA_SCALE if mla else GQA_SCALE
        nkv = 8 if mla else 4
        grp = nh // nkv
        for kv in range(nkv):
            if mla:
                S.dma("sp", self.kres[:, 0, :], scr["kTb"][kv], reads=[("kTb", kv, t) for t in range(5)], writes=["kres"])
                S.dma("sp", self.kres[0:64, 1, :], scr["krT"][0], reads=[("krT", t) for t in range(5)], writes=["kres"])
                S.dma("sp", self.vres[:], scr["vb"][:, kv * 128:(kv + 1) * 128].rearrange("(s p) d -> p s d", p=128),
                      reads=[("vb", kv, m) for m in range(18)], writes=["vres"])
            else:
                S.dma("sp", self.kres[:, 0, :], scr["kTa"][kv], reads=[("kTa", kv, t) for t in range(5)], writes=["kres"])
                S.dma("sp", self.vres[:], scr["va"][:, kv * 128:(kv + 1) * 128].rearrange("(s p) d -> p s d", p=128),
                      reads=[("va", m) for m in range(18)], writes=["vres"])
            for hh in range(grp):
                h = kv * grp + hh
                def body(h, ti, t0, n):
                    qn, qt = self.qres.next()
                    if mla:
                        S.dma("sp", qt[:, 0, 0:n], scr["qTbn"][h, :, t0:t0 + n], reads=[("qTbn", h, ti)], writes=[qn])
                        S.dma("sp", qt[0:64, 1, 0:n], scr["qTbr"][h, :, t0:t0 + n], reads=[("qTbr", h, ti)], writes=[qn])
                    else:
                        S.dma("sp", qt[:, 0, 0:n], scr["qTa"][h, :, t0:t0 + n], reads=[("qTa", h, ti)], writes=[qn])
                    nsc = 2 if ti == 0 else 18
                    on_, ot_ = self.psB.next()
                    dn_, dt_ = self.psB.next()
                    for sc in range(nsc):
                        pn, pt = self.psA.next()
                        if mla:
                            S.add("pe", lambda e, pt=pt, qt=qt, sc=sc: e.matmul(pt[:, 0:n], self.kres[:, 0, sc * 128:(sc + 1) * 128], qt[:, 0, 0:n], start=True, stop=False),
                                  reads=["kres", qn], writes=[pn])
                            S.add("pe", lambda e, pt=pt, qt=qt, sc=sc: e.matmul(pt[:, 0:n], self.kres[0:64, 1, sc * 128:(sc + 1) * 128], qt[0:64, 1, 0:n], start=False, stop=True),
                                  reads=["kres", qn], writes=[pn])
                        else:
                            S.add("pe", lambda e, pt=pt, qt=qt, sc=sc: e.matmul(pt[:, 0:n], self.kres[:, 0, sc * 128:(sc + 1) * 128], qt[:, 0, 0:n], start=True, stop=True),
                                  reads=["kres", qn], writes=[pn])
                        en, et = self.b16r.next()
                        S.add("act", lambda e, et=et, pt=pt: e.activation(et[:, 0:n], pt[:, 0:n], AF.Exp, scale=scale), reads=[pn], writes=[en])
                        S.add("pe", lambda e, et=et, sc=sc: e.matmul(ot_[:, 0:n], self.vres[:, sc, :], et[:, 0:n], start=(sc == 0), stop=(sc == nsc - 1)),
                              reads=[en, "vres"], writes=[on_])
                        S.add("pe", lambda e, et=et, sc=sc: e.matmul(dt_[:, 0:n], self.cm[:, 3, :], et[:, 0:n], start=(sc == 0), stop=(sc == nsc - 1)),
                              reads=[en, "cm"], writes=[dn_])
                    rn, rt = self.f32r.next()
                    S.add("dve", lambda e, rt=rt: e.reciprocal(out=rt[:, 0:n], in_=dt_[:, 0:n]), reads=[dn_], writes=[rn])
                    yn, yt = self.b16r.next()
                    S.add("dve", lambda e, rt=rt, yt=yt: e.tensor_tensor(out=yt[:, 0:n], in0=ot_[:, 0:n], in1=rt[:, 0:n], op=ALU.mult), reads=[on_, rn], writes=[yn])
                    yc = (16 + h) if mla else h
                    S.dma("sp", scr["yT"][yc, :, t0:t0 + n], yt[:, 0:n], reads=[yn], writes=[("yT", ti)])

                for ti, (t0, n) in enumerate(TT):
                    body(h, ti, t0, n)

    def fourier(self):
        S = self.S
        scr = self.scr
        inp = self.inp
        zf2 = scr["zfT"].rearrange("k p t -> (k p) t")
        for g in range(4):
            def epi1(grp, ti, ps, g=g):
                mi = grp[0]
                pn, pt = ps[0]
                bn, bt = self.b16r.next()
                S.add("act", lambda e: e.copy(out=bt[:, 0:512], in_=pt[:, 0:512]), reads=[pn], writes=[bn])
                if mi < 2:
                    r0 = mi * 128
                    S.dma("sp", scr["ABx"][r0:r0 + 128, g * 256:(g + 1) * 256], bt[:, 0:256], reads=[bn], writes=[("ABx", g, mi, 0)])
                    S.dma("sp", scr["ABx"][NCTX + r0:NCTX + r0 + 128, g * 256:(g + 1) * 256], bt[:, 256:512], reads=[bn], writes=[("ABx", g, mi, 1)])
                else:
                    r0 = (mi - 2) * 128
                    S.dma("sp", scr["ABl"][r0:r0 + 128, g * 256:(g + 1) * 256], bt[:, 0:256], reads=[bn], writes=[("ABl", g, mi, 0)])
                    S.dma("sp", scr["ABl"][SEQ + r0:SEQ + r0 + 128, g * 256:(g + 1) * 256], bt[:, 256:512], reads=[bn], writes=[("ABl", g, mi, 1)])

            self.gemm(256, [128] * 18, lambda mi, g=g: zf2[g * 256:(g + 1) * 256, mi * 128:(mi + 1) * 128], False,
                      lambda mi: [("zfT", t) for t in range(5)], [512], lambda ti: inp["dftC"], False, lambda ti: [], epi1)

        def epi2(grp, ti, ps):
            mi = grp[0]
            pn, pt = ps[0]
            t0 = NCTX + ti * 512
            self.evac_store(pn, pt, 128, 512, scr["yT"][24 + mi, :, t0:t0 + 512], ("yT", ti + 1))

        abl_keys = [("ABl", g, mi, s) for g in range(4) for mi in range(2, 18) for s in range(2)]
        self.gemm(2 * SEQ, [128] * 8, lambda mi: scr["ABl"][:, mi * 128:(mi + 1) * 128], False, lambda mi: abl_keys,
                  [512] * 4, lambda ti: inp["dftN"][:, ti * 512:(ti + 1) * 512], False, lambda ti: [], epi2, sets=[[0, 1], [2, 3]])

        def epi3(grp, ti, ps):
            mi = grp[0]
            pn, pt = ps[0]
            self.evac_store(pn, pt, 128, 256, scr["yT"][24 + mi, :, 0:256], ("yT", 0))

        abx_keys = [("ABx", g, mi, s) for g in range(4) for mi in range(2) for s in range(2)]
        self.gemm(2 * NCTX, [128] * 8, lambda mi: scr["ABx"][:, mi * 128:(mi + 1) * 128], False, lambda mi: abx_keys,
                  [256], lambda ti: inp["dftX"], False, lambda ti: [], epi3)

    def merge_out(self, l):
        S = self.S
        scr = self.scr
        inp = self.inp
        ntl = [n for (_, n) in TT]
        yT2 = scr["yT"].rearrange("k p t -> (k p) t")

        def epi_m(grp, ti, ps):
            oc = grp[0]
            t0, n = TT[ti]
            acc = None
            for s in range(3):
                pn, pt = ps[s]
                gn, gt = self.b16r.next()
                S.dma("sp", gt[:, 0:n], scr["gT"][s * 32 + oc, :, t0:t0 + n], reads=[("gT", s * 32 + oc, ti)], writes=[gn])
                xn, xt = self.f32r.next()
                S.add("dve", lambda e, xt=xt, pt=pt, gt=gt: e.tensor_tensor(out=xt[:, 0:n], in0=pt[:, 0:n], in1=gt[:, 0:n], op=ALU.mult), reads=[pn, gn], writes=[xn])
                if acc is None:
                    acc = (xn, xt)
                else:
                    an, at = acc
                    if s == 2:
                        on, ot = self.b16r.next()
                    else:
                        on, ot = self.f32r.next()
                    S.add("dve", lambda e, ot=ot, at=at, xt=xt: e.tensor_tensor(out=ot[:, 0:n], in0=at[:, 0:n], in1=xt[:, 0:n], op=ALU.add), reads=[an, xn], writes=[on])
                    acc = (on, ot)
            on, ot = acc
            S.dma("sp", scr["mT"][oc, :, t0:t0 + n], ot[:, 0:n], reads=[on], writes=[("mT", ti)])

        wbr = inp["w_br"][l]
        self.gemm(D, [128] * 32, lambda mi: wbr[mi], True, lambda mi: [],
                  ntl, lambda ti: yT2[:, TT[ti][0]:TT[ti][0] + TT[ti][1]], False, lambda ti: [("yT", ti)], epi_m,
                  segs=[(0, 16), (16, 24), (24, 32)], sets=[[0, 1], [2, 3], [4]], a_pre=True)
        mT2 = scr["mT"].rearrange("k p t -> (k p) t")
        self.gemm(D, [128] * 32, lambda mi: inp["w_out"][l][mi], True, lambda mi: [],
                  ntl, lambda ti: mT2[:, TT[ti][0]:TT[ti][0] + TT[ti][1]], False, lambda ti: [("mT", ti)],
                  lambda grp, ti, ps: self.resid_epi(grp[0], ti, ps[0], 2), sets=[[0, 1], [2, 3], [4]], a_pre=True)

    def resid_epi(self, oc, ti, p, j):
        S = self.S
        pn, pt = p
        t0, n = TT[ti]
        hT = self.scr["hT"]
        hn, ht = self.f32r.next()
        S.dma("sp", ht[:, 0:n], hT[oc, :, t0:t0 + n], reads=[("hT", ti), ("hTw", oc, ti)], writes=[hn])
        on, ot = self.f32r.next()
        S.add("dve", lambda e: e.scalar_tensor_tensor(out=ot[:, 0:n], in0=pt[:, 0:n], scalar=self.mvec(j, oc, ti), in1=ht[:, 0:n], op0=ALU.mult, op1=ALU.add),
              reads=[pn, hn, "modT"], writes=[on])
        S.dma("sp", hT[oc, :, t0:t0 + n], ot[:, 0:n], reads=[on, ("hT", ti)], writes=[("hTw", oc, ti)])

    def h_keys_done(self):
        S = self.S
        for ti in range(5):
            for oc in range(KC):
                pass
            S.dma("sp", self.scr["junk"][ti:ti + 1, :], self.zero[0:1, 0:64], reads=[("hTw", oc, ti) for oc in range(KC)] + ["zero"], writes=[("hT", ti), ("junk", ti)])

    def moe(self, l):
        S = self.S
        scr = self.scr
        inp = self.inp
        ntl = [n for (_, n) in TT]
        rps = {}

        def hook(ti, kc, on, ot, n):
            t0 = TT[ti][0]
            nsb = n // 128
            if kc is not None:
                if kc == 0:
                    rps[ti] = [self.psA.next() for _ in range(nsb)]
                for j in range(nsb):
                    rn, rt = rps[ti][j]
                    S.add("pe", lambda e, j=j, rt=rt, ot=ot, kc=kc: e.matmul(rt[:, 0:32], ot[:, j * 128:(j + 1) * 128], self.wrb[:, kc, :], start=(kc == 0), stop=(kc == KC - 1)),
                          reads=[on, "wrb"], writes=[rn])
                return
            HL = getattr(self, "hook_level", 9)
            for j in range(nsb):
                if HL < 2:
                    break
                rn, rt = rps[ti][j]
                ln, lt = self.sm.next()
                S.add("dve", lambda e, lt=lt, rt=rt: e.tensor_tensor(out=lt[:, 0:32], in0=rt[:, 0:32], in1=self.brB[:], op=ALU.add), reads=[rn, "brB"], writes=[ln])
                S.add("dve", lambda e, lt=lt: e.max(out=lt[:, 32:40], in_=lt[:, 0:32]), reads=[ln], writes=[ln])
                mn, mt_ = self.sm.next()
                S.add("dve", lambda e, lt=lt, mt_=mt_: e.tensor_scalar(mt_[:, 0:32], lt[:, 0:32], lt[:, 35:36], None, ALU.is_ge), reads=[ln], writes=[mn])
                S.add("act", lambda e, lt=lt: e.mul(out=lt[:, 40:41], in_=lt[:, 32:33], mul=-1.0), reads=[ln], writes=[ln])
                S.add("act", lambda e, lt=lt, mt_=mt_: e.activation(mt_[:, 32:64], lt[:, 0:32], AF.Exp, bias=lt[:, 40:41], scale=1.0), reads=[ln, mn], writes=[mn])
                S.add("dve", lambda e, mt_=mt_: e.tensor_tensor(out=mt_[:, 0:32], in0=mt_[:, 0:32], in1=mt_[:, 32:64], op=ALU.mult), reads=[mn], writes=[mn])
                S.add("dve", lambda e, lt=lt, mt_=mt_: e.tensor_reduce(out=lt[:, 41:42], in_=mt_[:, 0:32], axis=mybir.AxisListType.X, op=ALU.add), reads=[mn, ln], writes=[ln])
                S.add("dve", lambda e, lt=lt: e.reciprocal(out=lt[:, 42:43], in_=lt[:, 41:42]), reads=[ln], writes=[ln])
                S.add("dve", lambda e, lt=lt, mt_=mt_: e.tensor_scalar(mt_[:, 0:32], mt_[:, 0:32], lt[:, 42:43], None, ALU.mult), reads=[mn, ln], writes=[mn])
                if HL < 3:
                    continue
                tn, tq = self.sm.next()
                S.add("dve", lambda e, tq=tq, mt_=mt_: e.transpose(out=tq[:, 0:32], in_=mt_[:, 0:32]), reads=[mn], writes=[tn])
                bn, bq = self.b16r.next()
                S.add("act", lambda e, bq=bq, tq=tq: e.copy(out=bq[:, 0:32], in_=tq[:, 0:32]), reads=[tn], writes=[bn])
                if HL < 4:
                    continue
                for g in range(4):
                    c0 = t0 + j * 128 + g * 32
                    S.dma("sp", scr["combT"][:, c0:c0 + 32], tq[32 * g:32 * g + 32, 0:32], reads=[tn], writes=[("combT", ti, j, g)])
                    S.dma("sp", scr["actT"][128, 0:32, c0:c0 + 32], bq[32 * g:32 * g + 32, 0:32], reads=[bn, ("actT", 128, t0)], writes=[("actTc", ti, j, g)])

        self.norm_pass(scr["hT"], "hT", KC, scr["uT"], "uT", lambda ti, kc: self.sc[:, 1, kc, (1 if ti == 0 else 0):(2 if ti == 0 else 1)],
                       lambda ti, kc: self.mvec(3, kc, ti), ["modT", "scl"], 4, hook=(hook if "router" in self.parts else None))
        uT2 = scr["uT"].rearrange("k p t -> (k p) t")
        wgu = inp["w_gu"][l] if "gu" in self.parts else None

        def epi_gu(grp, ti, ps):
            e_ = grp[0] // 8
            jj = (grp[0] % 8) // 2
            t0, n = TT[ti]
            (gn, gp), (un, up) = ps
            cn, ct = self.f32r.next()
            S.dma("sp", ct[:, 0:n], scr["combT"][e_:e_ + 1, t0:t0 + n].to_broadcast([128, n]),
                  reads=[("combT", ti, j, g) for j in range(n // 128) for g in range(4)], writes=[cn])
            an, at = self.f32r.next()
            S.add("dve", lambda e: e.tensor_scalar(at[:, 0:n], gp[:, 0:n], self.bguT[:, grp[0]:grp[0] + 1], 7.0, ALU.add, ALU.min), reads=[gn, "bguT"], writes=[an])
            bn, bt = self.f32r.next()
            S.add("dve", lambda e: e.tensor_scalar(bt[:, 0:n], up[:, 0:n], self.bguT[:, grp[1]:grp[1] + 1], 7.0, ALU.add, ALU.min), reads=[un, "bguT"], writes=[bn])
            S.add("dve", lambda e: e.tensor_scalar(bt[:, 0:n], bt[:, 0:n], -7.0, 1.0, ALU.max, ALU.add), reads=[bn], writes=[bn])
            sn, st_ = self.f32r.next()
            S.add("act", lambda e: e.activation(st_[:, 0:n], at[:, 0:n], AF.Sigmoid, scale=1.702), reads=[an], writes=[sn])
            S.add("dve", lambda e: e.tensor_tensor(out=at[:, 0:n], in0=at[:, 0:n], in1=st_[:, 0:n], op=ALU.mult), reads=[an, sn], writes=[an])
            S.add("dve", lambda e: e.tensor_tensor(out=at[:, 0:n], in0=at[:, 0:n], in1=bt[:, 0:n], op=ALU.mult), reads=[an, bn], writes=[an])
            on, ot = self.b16r.next()
            S.add("dve", lambda e: e.tensor_tensor(out=ot[:, 0:n], in0=at[:, 0:n], in1=ct[:, 0:n], op=ALU.mult), reads=[an, cn], writes=[on])
            S.dma("sp", scr["actT"][e_ * 4 + jj, :, t0:t0 + n], ot[:, 0:n], reads=[on], writes=[("actT", e_ // 8, ti)])

        if "gu" in self.parts:
            self.gemm(D, [128] * 256, lambda mi: wgu[mi], True, lambda mi: [],
                      ntl, lambda ti: uT2[:, TT[ti][0]:TT[ti][0] + TT[ti][1]], False, lambda ti: [("uT", ti)], epi_gu,
                      group=2, sets=[[0, 1], [2, 3], [4]], a_pre=True)
        actT2 = scr["actT"].rearrange("k p t -> (k p) t")
        wdn = inp["w_dn"][l] if "dn" in self.parts else None
        for kg in (range(4) if "dn" in self.parts else ()):
            k0 = kg * 4096
            kk = 4096 + (128 if kg == 3 else 0)

            def bkeys(ti, kg=kg):
                ks = [("actT", kg, ti)]
                if kg == 3:
                    ks += [("actTc", ti, j, g) for j in range(TT[ti][1] // 128) for g in range(4)] + [("actT", 128, TT[ti][0])]
                return ks

            self.gemm(kk, [128] * 32, lambda mi, k0=k0, kk=kk: wdn[mi][:, k0 // 128:(k0 + kk) // 128, :], True, lambda mi: [],
                      ntl, lambda ti, k0=k0, kk=kk: actT2[k0:k0 + kk, TT[ti][0]:TT[ti][0] + TT[ti][1]], False, bkeys,
                      lambda grp, ti, ps: self.resid_epi(grp[0], ti, ps[0], 5), sets=[[0, 1], [2, 3], [4]], a_pre=True)

    def build(self, stop_after=None):
        S = self.S
        scr = self.scr
        self.consts()
        for ti, (t0, n) in enumerate(TT):
            for g0 in range(0, KC, 4):
                ln, lt = self.ldr.next()
                lv = lt[:, 0:4 * n].rearrange("p (k n) -> p k n", n=n)
                S.dma("sp", lv, self.inp["hT0"][g0:g0 + 4, :, t0:t0 + n].rearrange("k p n -> p k n"), writes=[ln])
                S.dma("sp", scr["hT"][g0:g0 + 4, :, t0:t0 + n].rearrange("k p n -> p k n"), lv, reads=[ln], writes=[("hT", ti)])
        for l in range(self.nlayers):
            if self.mode == "B":
                self.load_vecs(l)
                S.dma("sp", self.modT[:], self.inp["modTi"], writes=["modT"])
                self.derive()
            else:
                self.mod_phase(l)
                if self.mode == "A":
                    S.dma("sp", self.modTo, self.modT[:], reads=["modT"])
            if stop_after == "mod":
                break
            if self.mode != "B":
                self.norm_pass(scr["hT"], "hT", KC, scr["uT"], "uT", lambda ti, kc: self.sc[:, 0, kc, (1 if ti == 0 else 0):(2 if ti == 0 else 1)],
                               lambda ti, kc: self.mvec(0, kc, ti), ["modT", "scl"], 4)
                if stop_after == "norm1":
                    break
                self.inproj(l)
                if stop_after == "inproj":
                    break
                self.mla_up(l)
                if stop_after == "mla_up":
                    break
                self.attention(False)
                self.attention(True)
                if stop_after == "attn":
                    break
                self.fourier()
                if stop_after == "fourier":
                    break
                self.merge_out(l)
                self.h_keys_done()
                if stop_after == "mix":
                    break
            if self.mode != "A":
                self.moe(l)
                self.h_keys_done()
                if stop_after == "moe":
                    break
            S.add("dve", lambda e: e.memset(self.sm.tiles[0][1][:, 0:1], 0.0), reads=["modT", "scl", "vecs", "bguT", "brB", "wrb", "badaT"], writes=["modT_users", self.sm.tiles[0][0]])
        if stop_after is None and self.final and self.mode != "A":
            self.norm_pass(scr["hT"], "hT", KC, self.out, "out", lambda ti, kc: self.vecs[:, 3 * KC + kc:3 * KC + kc + 1], None, ["vecsF"], 4,
                           dst_f32=True, tiles=[1, 2, 3, 4], dst_off=NCTX)
        S.emit()
        return self.nc


def _fm(v, n):
    return np.ascontiguousarray(np.asarray(v, np.float32).reshape(n, 128).T)


def _consts():
    theta = 10000.0
    tok = np.arange(SEQ)
    row = (tok // 64).astype(np.float64)
    col = (tok % 64).astype(np.float64)

    def tab(rot):
        half = rot // 2
        q = half // 2
        inv = 1.0 / (theta ** (np.arange(0, half, 2, dtype=np.float64) / half))
        cos = np.ones((rot, T))
        sin = np.zeros((rot, T))
        for d in range(rot):
            pos = row if d < half else col
            dd = d % half
            f = dd % q
            ang = pos * inv[f]
            cos[d, NCTX:] = np.cos(ang)
            sin[d, NCTX:] = np.sin(ang) * (-1.0 if dd < q else 1.0)
        return cos, sin

    ropeA = np.zeros((4, 128, T), np.float32)
    c, s = tab(128)
    ropeA[0], ropeA[1] = c, s
    c, s = tab(64)
    ropeA[2, :64], ropeA[3, :64] = c, s
    cm = np.zeros((7, 128, 128), np.float32)
    cm[0] = 1.0 / 128.0
    for d in range(128):
        cm[1, d, (d + 32) % 64 + 64 * (d // 64)] = 1.0
    for d in range(64):
        cm[2, d, (d + 16) % 32 + 32 * (d // 32)] = 1.0
    cm[3] = 1.0
    cm[4] = 1.0 / 4096.0
    cm[5] = 1.0 / 1024.0
    cm[6] = 1.0 / 512.0
    k = np.arange(256)
    angc = 2 * np.pi * np.outer(k, k) / 256.0
    dftC = np.concatenate([np.cos(angc), -np.sin(angc)], axis=1) / 16.0

    def dn(N):
        kk = np.arange(N)
        a = 2 * np.pi * (np.outer(kk, kk) % N) / N
        return np.concatenate([np.cos(a), np.sin(a)], axis=0) / np.sqrt(N)

    bf = ml_dtypes.bfloat16
    return {
        "ropeA": ropeA.astype(bf), "cmat": cm.astype(bf), "identF": np.eye(128, dtype=np.float32),
        "dftC": dftC.astype(np.float32).astype(bf), "dftN": dn(SEQ).astype(np.float32).astype(bf),
        "dftX": dn(NCTX).astype(np.float32).astype(bf),
    }


def _pretile(W, nmi=None):
    K_, N_ = W.shape
    return np.ascontiguousarray(W.reshape(K_ // 128, 128, N_ // 128, 128).transpose(2, 1, 0, 3))


def _pretile_cols(W, tiles):
    K_ = W.shape[0]
    out = np.zeros((len(tiles), 128, K_ // 128, 128), np.float32)
    for i, (c0, m, _, _) in enumerate(tiles):
        out[i, :, :, :m] = W[:, c0:c0 + m].reshape(K_ // 128, 128, m).transpose(1, 0, 2)
    return out


def prep_shared(inputs, layers):
    g = {k: np.asarray(v) for k, v in inputs.items()}
    L = len(layers)
    sh = {}
    sh["w_ada"] = np.stack([_pretile(g["w_ada"][l]) for l in layers])
    sh["b_adaT"] = np.stack([_fm(g["b_ada"][l], 192) for l in layers])
    sh["nmixT"] = np.stack([_fm(g["norm_mix"][l], KC) for l in layers])
    sh["nffnT"] = np.stack([_fm(g["norm_ffn"][l], KC) for l in layers])
    sh["nfinT"] = _fm(g["norm_final"], KC)
    mt = _inproj_tiles()
    sh["w_in"] = np.stack([_pretile_cols(g["w_in"][l], mt) for l in layers])
    sh["w_v"] = np.stack([np.ascontiguousarray(g["w_in"][l][:, 512:1024]) for l in layers])
    sh["qnT"] = np.stack([_fm(g["gqa_q_norm"][l], 1) for l in layers])
    sh["knT"] = np.stack([_fm(g["gqa_k_norm"][l], 1) for l in layers])
    sh["mqnT"] = np.stack([_fm(g["mla_q_norm"][l], 8) for l in layers])
    sh["mkvnT"] = np.stack([_fm(g["mla_kv_norm"][l], 4) for l in layers])
    sh["w_uq"] = np.ascontiguousarray(g["mla_w_uq"][layers])
    sh["w_ukv"] = np.ascontiguousarray(g["mla_w_ukv"][layers])
    sh["w_br"] = np.stack([_pretile(np.concatenate([g["w_br_gqa"][l], g["w_br_mla"][l], g["w_br_fourier"][l]], axis=0)) for l in layers])
    sh["w_out"] = np.stack([_pretile(g["w_out"][l]) for l in layers])
    sh["w_router"] = np.ascontiguousarray(g["w_router"][layers])
    sh["b_routerB"] = np.stack([np.broadcast_to(g["b_router"][l][None, :], (128, NE)) for l in layers]).astype(np.float32)
    perm = np.concatenate([np.concatenate([2 * np.arange(j * 128, (j + 1) * 128), 2 * np.arange(j * 128, (j + 1) * 128) + 1]) for j in range(4)])
    sh["w_gu"] = np.stack([np.concatenate([_pretile(g["w_gate_up"][l][e][:, perm]) for e in range(NE)], axis=0) for l in layers])
    sh["b_guT"] = np.stack([np.ascontiguousarray(g["b_gate_up"][l][:, perm].reshape(NE * 8, 128).T) for l in layers])
    wds = []
    for i, l in enumerate(layers):
        wd = np.zeros((NE * 512 + 128, D), np.float32)
        wd[:NE * 512] = g["w_down"][l].reshape(NE * 512, D)
        wd[NE * 512:NE * 512 + NE] = g["b_down"][l]
        wds.append(_pretile(wd))
    sh["w_dn"] = np.stack(wds)
    sh.update(_consts())
    return sh


def prep_core(inputs, b):
    x = np.asarray(inputs["x"][b], np.float32)
    ctx = np.asarray(inputs["ctx"][b], np.float32)
    h = np.concatenate([ctx, x], axis=0)
    hT0 = np.ascontiguousarray(h.T).reshape(KC, 128, T)
    c2 = np.stack([np.asarray(inputs["c"][b], np.float32), np.asarray(inputs["c_ctx"], np.float32)], axis=1)
    cT = np.ascontiguousarray(c2.reshape(KC, 128, 2).transpose(1, 0, 2))
    return {"hT0": hT0, "cT": cT}


def kernel(**inputs):
    n = 8
    nc = Builder(NL).build()
    sh = prep_shared(inputs, list(range(NL)))
    in_maps = []
    for b in range(n):
        m = dict(sh)
        m.update(prep_core(inputs, b))
        in_maps.append(m)
    res = run_bass_kernel_spmd(nc, in_maps, core_ids=list(range(n)))
    out = np.empty((n, SEQ, D), np.float32)
    for b in range(n):
        o = res.results[b]["out"]
        out[b] = o.reshape(D, SEQ).T
    return out
```
